# Optimizing a Trainium2 kernel written in Bass

```python
import math
import jax, jax.numpy as jnp
from jax import lax
import numpy as np

D_MODEL = 2048
BATCH = 16
SEQ = 2048
DEPTH = 2

EPS = 1e-6
NEG_INF = -1e30
ROPE_BASE = 10000.0

RET_HEADS = 8
RET_DIM = 128
RET_WIDTH = RET_HEADS * RET_DIM
RET_CHUNK = 128
LRU_WIDTH = D_MODEL // 2
LRU_BLOCKS = 8
LRU_BLOCK_DIM = LRU_WIDTH // LRU_BLOCKS
LRU_CONV = 4
LRU_C = 8.0
E_SPLITS = (RET_WIDTH, 2 * RET_WIDTH, 3 * RET_WIDTH, 4 * RET_WIDTH, 4 * RET_WIDTH + LRU_WIDTH)
E_IN = 4 * RET_WIDTH + 2 * LRU_WIDTH
E_MIX = RET_WIDTH + LRU_WIDTH

ATT_HEADS = 16
ATT_DIM = D_MODEL // ATT_HEADS
KV_HEADS = 4
IDX_HEADS = 16
IDX_DIM = 64
TOPK_MAX = 256
Q_BLOCK = 128
O_Q = ATT_HEADS * ATT_DIM
O_KV = KV_HEADS * ATT_DIM
O_QI = IDX_HEADS * IDX_DIM
O_SPLITS = (O_Q, O_Q + O_KV, O_Q + 2 * O_KV, O_Q + 2 * O_KV + O_QI, O_Q + 2 * O_KV + O_QI + IDX_DIM)
O_IN = O_SPLITS[-1] + IDX_HEADS

REL_BUCKETS = 32
REL_MAX_DIST = 128

D_FF = 5632
FFN_CONV = 3

N_EVEN = (DEPTH + 1) // 2
N_ODD = DEPTH // 2

kernel_name = 'hybrid_retention_rglru_dsa_convffn'


def rmsnorm(x, g):
    xf = x.astype(jnp.float32)
    y = xf * lax.rsqrt(jnp.mean(xf * xf, axis=-1, keepdims=True) + EPS)
    return (y * g.astype(jnp.float32)).astype(x.dtype)


def causal_dwconv(x, w, b):
    width = w.shape[0]
    S = x.shape[1]
    xp = jnp.pad(x, ((0, 0), (width - 1, 0), (0, 0)))
    out = b
    for j in range(width):
        out = out + w[j] * xp[:, j:j + S]
    return out


def rotary(x, pos):
    half = x.shape[-1] // 2
    freqs = ROPE_BASE ** (-jnp.arange(half, dtype=jnp.float32) / half)
    ang = pos.astype(jnp.float32)[:, None] * freqs[None, :]
    cos = jnp.cos(ang)[None, :, None, :]
    sin = jnp.sin(ang)[None, :, None, :]
    x1, x2 = x[..., :half], x[..., half:]
    return jnp.concatenate([x1 * cos - x2 * sin, x2 * cos + x1 * sin], axis=-1)


def retention_chunkwise(q, k, v):
    B, S, H, Dh = q.shape
    C = RET_CHUNK
    NC = S // C
    log_g = jnp.log1p(-jnp.exp2(-5.0 - jnp.arange(H, dtype=jnp.float32)))
    pos = jnp.arange(C, dtype=jnp.float32)
    diff = pos[:, None] - pos[None, :]
    inner_decay = jnp.where(diff[None] >= 0, jnp.exp(jnp.maximum(diff, 0.0)[None] * log_g[:, None, None]), 0.0)
    q_decay = jnp.exp((pos[None, :] + 1.0) * log_g[:, None])
    k_decay = jnp.exp((C - 1.0 - pos[None, :]) * log_g[:, None])
    chunk_decay = jnp.exp(C * log_g)
    qc = q.reshape(B, NC, C, H, Dh)
    kc = k.reshape(B, NC, C, H, Dh)
    vc = v.reshape(B, NC, C, H, Dh)
    scores = jnp.einsum('bnihd,bnjhd->bnhij', qc, kc) * inner_decay
    y_inner = jnp.einsum('bnhij,bnjhd->bnihd', scores, vc)
    kv = jnp.einsum('bnjhd,hj,bnjhe->bnhde', kc, k_decay, vc)

    def step(state, kv_n):
        return chunk_decay[None, :, None, None] * state + kv_n, state

    _, prev = lax.scan(step, jnp.zeros((B, H, Dh, Dh), jnp.float32), jnp.moveaxis(kv, 1, 0))
    prev = jnp.moveaxis(prev, 0, 1)
    y_cross = jnp.einsum('bnihd,hi,bnhde->bnihe', qc, q_decay, prev)
    return (y_inner + y_cross).reshape(B, S, H, Dh)


def rg_lru(x, wa, ba, wx, bx, lam):
    B, S, W = x.shape
    xf = x.astype(jnp.float32)
    xb = xf.reshape(B, S, LRU_BLOCKS, LRU_BLOCK_DIM)
    r = jax.nn.sigmoid(jnp.einsum('bsnd,nde->bsne', xb, wa).reshape(B, S, W) + ba)
    i = jax.nn.sigmoid(jnp.einsum('bsnd,nde->bsne', xb, wx).reshape(B, S, W) + bx)
    log_a = -LRU_C * r * jax.nn.softplus(-lam)
    a = jnp.exp(log_a)
    mult = jnp.sqrt(jnp.maximum(-jnp.expm1(2.0 * log_a), 0.0))
    b = mult * (i * xf)

    def combine(c1, c2):
        a1, b1 = c1
        a2, b2 = c2
        return a1 * a2, a2 * b1 + b2

    _, h = lax.associative_scan(combine, (a, b), axis=1)
    return h


def hybrid_even_mixer(h, w_in, ret_gn, conv_w, conv_b, wa, ba, wx, bx, lam, w_out):
    B, S, _ = h.shape
    proj = h @ w_in
    q, k, v, g, xr, yr = jnp.split(proj, E_SPLITS, axis=-1)
    pos = jnp.arange(S)
    q = rotary(q.astype(jnp.float32).reshape(B, S, RET_HEADS, RET_DIM), pos)
    k = rotary(k.astype(jnp.float32).reshape(B, S, RET_HEADS, RET_DIM), pos) * (RET_DIM ** -0.5)
    v = v.astype(jnp.float32).reshape(B, S, RET_HEADS, RET_DIM)
    y = retention_chunkwise(q, k, v)
    y = y * lax.rsqrt(jnp.mean(y * y, axis=-1, keepdims=True) + EPS)
    y = y.reshape(B, S, RET_WIDTH) * ret_gn.astype(jnp.float32)
    ret_out = y * jax.nn.silu(g.astype(jnp.float32))
    xc = causal_dwconv(xr, conv_w, conv_b)
    lru_out = rg_lru(xc, wa, ba, wx, bx, lam) * jax.nn.gelu(yr.astype(jnp.float32))
    mixed = jnp.concatenate([ret_out, lru_out], axis=-1).astype(h.dtype)
    return mixed @ w_out


def t5_bucket(rel):
    n = jnp.maximum(rel, 0)
    max_exact = REL_BUCKETS // 2
    nf = jnp.maximum(n, max_exact).astype(jnp.float32)
    large = max_exact + (jnp.log(nf / max_exact) / math.log(REL_MAX_DIST / max_exact)
                         * (REL_BUCKETS - max_exact)).astype(jnp.int32)
    large = jnp.minimum(large, REL_BUCKETS - 1)
    return jnp.where(n < max_exact, n, large)


def dsa_attention(h, w_in, w_out, rel_bias):
    B, S, _ = h.shape
    topk = min(TOPK_MAX, S // 4)
    nb = S // Q_BLOCK
    G = ATT_HEADS // KV_HEADS
    proj = h @ w_in
    q, k, v, qi, ki, wi = jnp.split(proj, O_SPLITS, axis=-1)
    q = q.reshape(B, S, KV_HEADS, G, ATT_DIM)
    k = k.reshape(B, S, KV_HEADS, ATT_DIM)
    v = v.reshape(B, S, KV_HEADS, ATT_DIM)
    qi = qi.reshape(B, S, IDX_HEADS, IDX_DIM)
    wi = wi * (IDX_HEADS ** -0.5 * IDX_DIM ** -0.5)
    kpos = jnp.arange(S)

    def to_blocks(t):
        return jnp.moveaxis(t.reshape((B, nb, Q_BLOCK) + t.shape[2:]), 1, 0)

    def attend_block(blk):
        qb, qib, wib, qpos = blk
        s = jax.nn.relu(jnp.einsum('bqhd,bsd->bqhs', qib, ki).astype(jnp.float32))
        score = jnp.einsum('bqhs,bqh->bqs', s, wib.astype(jnp.float32))
        score = jnp.where(kpos[None, None, :] <= qpos[None, :, None], score, NEG_INF)
        _, idx = lax.top_k(score, topk)
        kg = jax.vmap(lambda kk, ii: kk[ii])(k, idx)
        vg = jax.vmap(lambda vv, ii: vv[ii])(v, idx)
        logits = jnp.einsum('bqkgd,bqjkd->bqkgj', qb, kg).astype(jnp.float32) * (ATT_DIM ** -0.5)
        bias = rel_bias[t5_bucket(qpos[None, :, None] - idx)]
        bias = jnp.transpose(bias.reshape(B, Q_BLOCK, topk, KV_HEADS, G), (0, 1, 3, 4, 2))
        valid = (idx <= qpos[None, :, None])[:, :, None, None, :]
        logits = jnp.where(valid, logits + bias.astype(jnp.float32), NEG_INF)
        p = jax.nn.softmax(logits, axis=-1)
        out = jnp.einsum('bqkgj,bqjkd->bqkgd', p.astype(vg.dtype), vg)
        return out.reshape(B, Q_BLOCK, O_Q)

    qpos_blocks = jnp.arange(S).reshape(nb, Q_BLOCK)
    out = lax.map(attend_block, (to_blocks(q), to_blocks(qi), to_blocks(wi), qpos_blocks))
    out = jnp.moveaxis(out, 0, 1).reshape(B, S, O_Q)
    return out @ w_out


def conv_ffn(h, w_gu, conv_w, conv_b, w_down):
    gu = h @ w_gu
    g, u = jnp.split(gu, [D_FF], axis=-1)
    g = causal_dwconv(g, conv_w, conv_b)
    return (jax.nn.silu(g) * u) @ w_down


def setup_inputs(seed: int = 0) -> dict:
    key = jax.random.key(seed)
    ks = jax.random.split(key, 21)
    f32 = jnp.float32

    def nrm(k, shape, fan_in):
        return jax.random.normal(k, shape, f32) * (fan_in ** -0.5)

    def gain(k, shape):
        return 1.0 + 0.02 * jax.random.normal(k, shape, f32)

    def small(k, shape):
        return 0.02 * jax.random.normal(k, shape, f32)

    u = jax.random.uniform(ks[11], (N_EVEN, LRU_WIDTH), f32, 0.9, 0.999)
    s = u ** (1.0 / LRU_C)
    e_lambda = jnp.log(s) - jnp.log1p(-s)
    return {
        'x': jax.random.normal(ks[0], (BATCH, SEQ, D_MODEL), f32),
        'norm_mix': gain(ks[1], (DEPTH, D_MODEL)),
        'norm_ffn': gain(ks[2], (DEPTH, D_MODEL)),
        'e_w_in': nrm(ks[3], (N_EVEN, D_MODEL, E_IN), D_MODEL),
        'e_ret_gn': gain(ks[4], (N_EVEN, RET_WIDTH)),
        'e_conv_w': nrm(ks[5], (N_EVEN, LRU_CONV, LRU_WIDTH), LRU_CONV),
        'e_conv_b': small(ks[6], (N_EVEN, LRU_WIDTH)),
        'e_gate_a_w': nrm(ks[7], (N_EVEN, LRU_BLOCKS, LRU_BLOCK_DIM, LRU_BLOCK_DIM), LRU_BLOCK_DIM),
        'e_gate_a_b': small(ks[8], (N_EVEN, LRU_WIDTH)),
        'e_gate_x_w': nrm(ks[9], (N_EVEN, LRU_BLOCKS, LRU_BLOCK_DIM, LRU_BLOCK_DIM), LRU_BLOCK_DIM),
        'e_gate_x_b': small(ks[10], (N_EVEN, LRU_WIDTH)),
        'e_lambda': e_lambda,
        'e_w_out': nrm(ks[12], (N_EVEN, E_MIX, D_MODEL), E_MIX),
        'o_w_in': nrm(ks[13], (N_ODD, D_MODEL, O_IN), D_MODEL),
        'o_w_out': nrm(ks[14], (N_ODD, O_Q, D_MODEL), O_Q),
        'rel_bias': 0.5 * jax.random.normal(ks[15], (REL_BUCKETS, ATT_HEADS), f32),
        'ffn_w_gu': nrm(ks[16], (DEPTH, D_MODEL, 2 * D_FF), D_MODEL),
        'ffn_conv_w': nrm(ks[17], (DEPTH, FFN_CONV, D_FF), FFN_CONV),
        'ffn_conv_b': small(ks[18], (DEPTH, D_FF)),
        'ffn_w_down': nrm(ks[19], (DEPTH, D_FF, D_MODEL), D_FF),
        'final_norm': gain(ks[20], (D_MODEL,)),
    }


def reference(x, norm_mix, norm_ffn, e_w_in, e_ret_gn, e_conv_w, e_conv_b, e_gate_a_w, e_gate_a_b,
              e_gate_x_w, e_gate_x_b, e_lambda, e_w_out, o_w_in, o_w_out, rel_bias,
              ffn_w_gu, ffn_conv_w, ffn_conv_b, ffn_w_down, final_norm):
    for layer in range(DEPTH):
        h = rmsnorm(x, norm_mix[layer])
        if layer % 2 == 0:
            e = layer // 2
            mix = hybrid_even_mixer(h, e_w_in[e], e_ret_gn[e], e_conv_w[e], e_conv_b[e],
                                    e_gate_a_w[e], e_gate_a_b[e], e_gate_x_w[e], e_gate_x_b[e],
                                    e_lambda[e], e_w_out[e])
        else:
            o = layer // 2
            mix = dsa_attention(h, o_w_in[o], o_w_out[o], rel_bias)
        x = x + mix.astype(x.dtype)
        h = rmsnorm(x, norm_ffn[layer])
        x = x + conv_ffn(h, ffn_w_gu[layer], ffn_conv_w[layer], ffn_conv_b[layer], ffn_w_down[layer]).astype(x.dtype)
    return rmsnorm(x, final_norm)
```

```python
import math
import os
import numpy as np
import concourse.bass as bass
import concourse.mybir as mybir
from concourse.bass_utils import run_bass_kernel_spmd

ACT = mybir.ActivationFunctionType
ALU = mybir.AluOpType
F32 = mybir.dt.float32
BF16 = mybir.dt.bfloat16

CELL = 256
STRICT = bool(os.environ.get('K_STRICT'))
RAW_WINDOW = 2
EPOCH = 30000
ENGS = ('pe', 'act', 'dve', 'pool', 'sp')


class Op:
    __slots__ = ('eng', 'fn', 'deps', 'signal', 'sig', 'is_dma', 'chan', 'dval', 'idx')

    def __init__(self, eng, fn, is_dma=False, chan=None):
        self.eng = eng
        self.fn = fn
        self.deps = []
        self.signal = False
        self.sig = 0
        self.is_dma = is_dma
        self.chan = chan
        self.dval = 0
        self.idx = 0


class Prog:
    def __init__(self, nc):
        self.nc = nc
        self.ops = {e: [] for e in ENGS}
        self.cells = {}
        self.chan_last = {}
        self.chan_cnt = {}
        self.n_ops = 0

    def _cells(self, ap):
        if isinstance(ap, str):
            return [('k', ap)]
        t = ap.tensor
        if 'DRam' in type(t).__name__:
            return []
        esz = mybir.dt.size(ap.dtype)
        pat = ap.ap
        pstep = pat[0][0]
        off = ap.offset
        if pstep > 0:
            off = off % pstep
        lo = off
        hi = off
        for (st, cnt) in pat[1:]:
            if st >= 0:
                hi += st * (cnt - 1)
            else:
                lo += st * (cnt - 1)
        lo_b = lo * esz
        hi_b = (hi + 1) * esz
        name = t.name
        if name.startswith('psb'):
            return [(name, 0)]
        return [(name, c) for c in range(lo_b // CELL, (hi_b - 1) // CELL + 1)]

    def add(self, eng, fn, reads=(), writes=(), chan=None):
        is_dma = chan is not None
        op = Op(eng, fn, is_dma, chan)
        self.n_ops += 1
        deps = {}

        def dep(d):
            if d is None or d is op:
                return
            if d.is_dma:
                deps[('dma', id(d))] = d
            else:
                k = ('c', d.eng)
                o = deps.get(k)
                if o is None or d.idx > o.idx:
                    deps[k] = d

        rcells = []
        for r in reads:
            if r is not None and not isinstance(r, (int, float)):
                rcells.extend(self._cells(r))
        wcells = []
        for w in writes:
            wcells.extend(self._cells(w))
        cells = self.cells
        nxt = len(self.ops[eng])
        for c in rcells:
            rec = cells.get(c)
            if rec is not None:
                w = rec[0]
                if w is not None and (STRICT or w.is_dma or is_dma or w.eng != eng or nxt - w.idx <= RAW_WINDOW):
                    dep(w)
                if c[0].startswith('psb'):
                    for e2, r in rec[1].items():
                        if e2 != eng:
                            dep(r)
        for c in wcells:
            rec = cells.get(c)
            if rec is not None:
                w = rec[0]
                if w is not None and (w.is_dma or is_dma or w.eng != eng or (STRICT and eng != 'pe')):
                    dep(w)
                for e2, r in rec[1].items():
                    if e2 != eng or is_dma or (STRICT and eng != 'pe'):
                        dep(r)
                for r in rec[2]:
                    dep(r)
        if is_dma:
            prev = self.chan_last.get(chan)
            if prev is not None:
                dep(prev)
            self.chan_last[chan] = op
            n = self.chan_cnt.get(chan, 0) + 1
            self.chan_cnt[chan] = n
            op.dval = 16 * n
        lst = self.ops[eng]
        op.idx = len(lst)
        lst.append(op)
        for d in deps.values():
            if not d.is_dma:
                d.signal = True
            op.deps.append(d)
        for c in wcells:
            cells[c] = [op, {}, []]
        for c in rcells:
            rec = cells.get(c)
            if rec is None:
                rec = [None, {}, []]
                cells[c] = rec
            if is_dma:
                rec[2].append(op)
            else:
                rec[1][eng] = op
        return op

    def emit(self, final_chans=()):
        nc = self.nc
        from contextlib import ExitStack
        with ExitStack() as es:
            nsig = {}
            for e in ENGS:
                n = 0
                for op in self.ops[e]:
                    if op.signal and not op.is_dma:
                        n += 1
                        op.sig = n
                nsig[e] = n
            sems = {}
            for e in ENGS:
                ne = max((nsig[e] + EPOCH - 1) // EPOCH, 1)
                sems[e] = [es.enter_context(nc.semaphore(f'c_{e}_{i}')) for i in range(ne)]
            chans = {}
            for ch in self.chan_cnt:
                chans[ch] = es.enter_context(nc.semaphore(f'd_{ch}'))
            block = es.enter_context(nc.Block())

            def make(e):
                def body(eng):
                    waited_c = {}
                    waited_d = {}
                    for op in self.ops[e]:
                        for d in op.deps:
                            if d.is_dma:
                                if waited_d.get(d.chan, 0) < d.dval:
                                    eng.wait_ge(chans[d.chan], d.dval)
                                    waited_d[d.chan] = d.dval
                            else:
                                if waited_c.get(d.eng, 0) < d.sig:
                                    ep = (d.sig - 1) // EPOCH
                                    eng.wait_ge(sems[d.eng][ep], (d.sig - 1) % EPOCH + 1)
                                    waited_c[d.eng] = d.sig
                        ins = op.fn(eng)
                        if op.is_dma:
                            ins.then_inc(chans[op.chan], 16)
                        elif op.signal:
                            ep = (op.sig - 1) // EPOCH
                            ins.then_inc(sems[e][ep], 1)
                    if e == 'sp':
                        for ch in final_chans:
                            eng.wait_ge(chans[ch], 16 * self.chan_cnt[ch])
                return body

            block.tensor(make('pe'))
            block.scalar(make('act'))
            block.vector(make('dve'))
            block.gpsimd(make('pool'))
            block.sync(make('sp'))


D = 2048
S = 2048
NSEQ = 2
T = 512
NT = S // T
NSUB = T // 128
KC = D // 128
E_IN = 6144
O_IN = 4176
DFF = 5632
FC = DFF // 128
EPS = 1e-6
NSLOT = 4
SLOT = 4096
NIT = 22
NEG = -1e30

SP_FIELDS = [('nmix', 32), ('nffn', 32), ('fnorm', 16), ('gn', 8), ('cw', 32), ('cb', 8), ('ba', 8),
             ('bx', 8), ('lam', 8), ('fw', 2 * FC * 3), ('fb', 2 * FC)]
SP_OFF = {}
_o = 0
for _n, _w in SP_FIELDS:
    SP_OFF[_n] = _o
    _o += _w
SP_W = _o
CS_FIELDS = [('ident', 128), ('triu', 128), ('perm', 128), ('negm', 128), ('qdec', 1024), ('kdec', 1024),
             ('c31', 16), ('pow2', NIT)]
CS_OFF = {}
_o = 0
for _n, _w in CS_FIELDS:
    CS_OFF[_n] = _o
    _o += _w
CS_W = _o

GAMMA = [1.0 - 2.0 ** (-5.0 - h) for h in range(8)]
CDEC = [g ** 128 for g in GAMMA]

N_EIN = E_IN // 256
N_EOUT = 8
N_GU = (2 * DFF) // 256
N_DOWN = 32
N_OIN = 17
N_OOUT = 8


def build_program(nseq=NSEQ, debug_stage=None, stop=None, ntiles=None):
    NTL = NT if ntiles is None else ntiles
    nc = bass.Bass("TRN2", target_bir_lowering=False)
    P = Prog(nc)

    def din(name, shape, dt=F32):
        return nc.dram_tensor(name, list(shape), dt, kind="ExternalInput").ap()

    x_in = din("x", [nseq, S, D])
    w_ein = din("e_w_in", [D, E_IN])
    w_eout = din("e_w_out", [D, D])
    w_oin = din("o_w_in", [D, O_IN])
    w_oout = din("o_w_out", [D, D])
    w_gu = din("ffn_w_gu", [2, D, 2 * DFF])
    w_dn = din("ffn_w_down", [2, DFF, D])
    sp_in = din("smallp", [128, SP_W])
    cs_in = din("consts", [128, CS_W])
    gates_in = din("gates", [128, 2, 8, 128])
    rot_in = din("rot", [2, 128, S])
    bt_in = din("bt", [16, 128, 256])
    out = nc.dram_tensor("out", [nseq, S, D], F32, kind="ExternalOutput").ap()

    def dscr(name, shape, dt):
        return nc.dram_tensor(name, list(shape), dt, kind="Internal").ap()

    s_ein = dscr("s_ein", [N_EIN, 128, SLOT], BF16)
    s_eout = dscr("s_eout", [N_EOUT, 128, SLOT], BF16)
    s_oin = dscr("s_oin", [N_OIN, 128, SLOT], BF16)
    s_oout = dscr("s_oout", [N_OOUT, 128, SLOT], BF16)
    s_gu = dscr("s_gu", [2, N_GU, 128, SLOT], BF16)
    s_dn = dscr("s_dn", [2, N_DOWN, 128, 22 * 128], BF16)
    if debug_stage == 'A':
        x2s = nc.dram_tensor("x2s", [nseq, NT, 128, KC * T], F32, kind="ExternalOutput").ap()
    else:
        x2s = dscr("x2s", [nseq, NT, 128, KC * T], F32)

    sb = nc.alloc_sbuf_tensor
    ring = sb("ring", [128, NSLOT, SLOT], BF16)
    xres = sb("xres", [128, KC, T], F32)
    spk = sb("spk", [128, SP_W], F32)
    cst = sb("cst", [128, CS_W], F32)
    identb = sb("identb", [128, 128], BF16)
    permb = sb("permb", [128, 128], BF16)
    onesb = sb("onesb", [128, 128], BF16)
    trib = sb("trib", [128, 128], BF16)
    gatesb = sb("gatesb", [128, 2, 8, 128], BF16)
    clam = sb("clam", [128, 16], F32)
    ARENA = 122 * 1024
    arena = sb("arena", [128, ARENA // 2], BF16)
    psb = [nc.alloc_psum_tensor(f"psb{i}", [128, 512], F32) for i in range(8)]

    def spf(name, lo=0, hi=None):
        o = SP_OFF[name]
        w = dict(SP_FIELDS)[name]
        hi = w if hi is None else hi
        return spk[:, o + lo:o + hi]

    def csf(name, lo=0, hi=None):
        o = CS_OFF[name]
        w = dict(CS_FIELDS)[name]
        hi = w if hi is None else hi
        return cst[:, o + lo:o + hi]

    ident = csf('ident')

    class Arena:
        def __init__(self):
            self.top = 0

        def alloc(self, shape, dt):
            n = 1
            for s_ in shape:
                n *= s_
            esz = mybir.dt.size(dt)
            nbytes = ((n * esz + 255) // 256) * 256
            lo = self.top
            self.top += nbytes
            assert self.top <= ARENA, f"arena overflow {self.top}"
            v = arena[:, lo // 2:(lo + n * esz) // 2]
            if dt != BF16:
                v = v.bitcast(dt)
            if len(shape) == 2:
                return v.rearrange("p (a b) -> p a b", a=shape[0])
            if len(shape) == 3:
                return v.rearrange("p (a b c) -> p a b c", a=shape[0], b=shape[1])
            return v

        def mark(self):
            return self.top

        def reset(self, m):
            self.top = m

    ar = Arena()

    class Rot:
        def __init__(self, items):
            self.items = items
            self.i = 0

        def next(self):
            v = self.items[self.i % len(self.items)]
            self.i += 1
            return v

    psum = Rot(psb)

    def mm(o, lhsT, rhs, start=True, stop=True):
        P.add('pe', lambda e: e.matmul(o, lhsT=lhsT, rhs=rhs, start=start, stop=stop), reads=[lhsT, rhs], writes=[o])

    def tr(o, in_, idn):
        P.add('pe', lambda e: e.transpose(o, in_, idn), reads=[in_, idn], writes=[o])

    def act(o, in_, func, bias=None, scale=None):
        kw = {}
        if bias is not None:
            kw['bias'] = bias
        if scale is not None:
            kw['scale'] = scale
        P.add('act', lambda e: e.activation(out=o, in_=in_, func=func, **kw), reads=[in_, bias, scale], writes=[o])

    def tt(eng, o, a, b, op):
        P.add(eng, lambda e: e.tensor_tensor(out=o, in0=a, in1=b, op=op), reads=[a, b], writes=[o])

    def ts(eng, o, a, s1, s2, op0, op1=None, accum=None):
        kw = {}
        if op1 is not None:
            kw['op1'] = op1
        if accum is not None:
            kw['accum_out'] = accum
        wr = [o] + ([accum] if accum is not None else [])
        P.add(eng, lambda e: e.tensor_scalar(out=o, in0=a, scalar1=s1, scalar2=s2, op0=op0, **kw), reads=[a, s1, s2], writes=wr)

    def stt(eng, o, a, sc, b, op0, op1):
        P.add(eng, lambda e: e.scalar_tensor_tensor(out=o, in0=a, scalar=sc, in1=b, op0=op0, op1=op1), reads=[a, sc, b], writes=[o])

    def cp(eng, o, a):
        if eng == 'act':
            act(o, a, ACT.Copy)
        else:
            P.add(eng, lambda e: e.tensor_copy(out=o, in_=a), reads=[a], writes=[o])

    def memset(eng, o, val):
        P.add(eng, lambda e: e.memset(o, val), writes=[o])

    def recip(o, a):
        P.add('dve', lambda e: e.reciprocal(out=o, in_=a), reads=[a], writes=[o])

    dma_rr = {'n': 0}

    def dma(q, o, i, reads=(), writes=(), chan=None):
        if chan is None:
            chan = f"g{dma_rr['n'] % 6}"
            dma_rr['n'] += 1
        P.add(q, lambda e: e.dma_start(out=o, in_=i), reads=list(reads), writes=list(writes), chan=chan)
        return chan

    dma('pool', spk[:], sp_in, writes=[spk[:]])
    dma('pool', cst[:], cs_in, writes=[cst[:]])
    m0 = ar.mark()
    gst = ar.alloc([2 * 8, 128], F32)
    dma('pool', gst, gates_in.rearrange("p a n e -> p (a n) e"), writes=[gst])
    cp('act', identb[:], ident)
    cp('dve', permb[:], csf('perm'))
    cp('dve', trib[:], csf('triu'))
    memset('dve', onesb[:], 1.0)
    cp('act', gatesb[:].rearrange("p a n e -> p (a n) e"), gst)
    act(clam[:, 0:8], spf('lam'), ACT.Exp, scale=-1.0)
    act(clam[:, 0:8], clam[:, 0:8], ACT.Ln, bias=1.0)
    ts('dve', clam[:, 8:16], clam[:, 0:8], -16.0, None, ALU.mult)
    ts('dve', clam[:, 0:8], clam[:, 0:8], -8.0, None, ALU.mult)
    ar.reset(m0)

    cast_done = set()
    wcnt = {'n': 0, 'c': 0}

    def cast_unit(key, dst, src):
        if key in cast_done:
            return
        cast_done.add(key)
        ch = f"cast{wcnt['c'] % 8}"
        wcnt['c'] += 1
        P.add('pool', lambda e: e.dma_start(out=dst, in_=src), writes=[key], chan=ch)

    def load_unit(key, src_scr, shape3):
        slot = wcnt['n'] % NSLOT
        wcnt['n'] += 1
        a, b = shape3
        dst = ring[:, slot, 0:a * b]
        P.add('sp', lambda e: e.dma_start(out=dst, in_=src_scr), reads=[key], writes=[dst], chan=f"w{slot}")
        return dst.rearrange("p (a b) -> p a b", a=a)

    def unit_A(name, wsrc, scr, u):
        key = f"{name}:{u}"
        src = wsrc[:, u * 256:(u + 1) * 256].rearrange("(k p) j -> p k j", p=128)
        cast_unit(key, scr[u].rearrange("p (k j) -> p k j", k=KC), src)
        return load_unit(key, scr[u], (KC, 256))

    def unit_oin_last():
        key = "oin:16"
        if key not in cast_done:
            cast_done.add(key)
            dstc = s_oin[16].rearrange("p (k j) -> p k j", k=KC)
            ki_src = w_oin[:, 4096:4160].rearrange("(k p) j -> p k j", p=128)
            wi_src = w_oin[:, 4160:4176].rearrange("(k p) j -> p k j", p=128)
            P.add('pool', lambda e: e.dma_start(out=dstc[:, :, 0:64], in_=ki_src), writes=[key + 'a'], chan="castx0")
            P.add('pool', lambda e: e.dma_start(out=dstc[:, :, 64:128], in_=ki_src), writes=[key + 'b'], chan="castx1")
            P.add('pool', lambda e: e.dma_start(out=dstc[:, :, 128:144], in_=wi_src), writes=[key + 'c'], chan="castx2")
        slot = wcnt['n'] % NSLOT
        wcnt['n'] += 1
        dst = ring[:, slot, :].rearrange("p (a b) -> p a b", a=KC)
        srcv = s_oin[16].rearrange("p (k j) -> p k j", k=KC)
        P.add('sp', lambda e: e.dma_start(out=dst[:, :, 0:144], in_=srcv[:, :, 0:144]), reads=[key + 'a', key + 'b', key + 'c'],
              writes=[dst[:, :, 0:144]], chan=f"w{slot}")
        return dst

    def unit_down(l, m, hf):
        key = f"dn{l}:{m}:{hf}"
        src = w_dn[l, hf * 22 * 128:(hf + 1) * 22 * 128, m * 128:(m + 1) * 128].rearrange("(c p) j -> p c j", p=128)
        cast_unit(key, s_dn[l, m * 2 + hf].rearrange("p (c j) -> p c j", c=22), src)
        return load_unit(key, s_dn[l, m * 2 + hf], (22, 128))

    def rmsnorm(gain, hT, scr):
        ss = psum.next()
        for c in range(KC):
            sq = scr['sq'][c % 2]
            act(sq, xres[:, c, :], ACT.Square)
            mm(ss[:], onesb[:], sq, start=(c == 0), stop=(c == KC - 1))
        rs = scr['rstd']
        ts('dve', rs, ss[:], 1.0 / D, EPS, ALU.mult, ALU.add)
        act(rs, rs, ACT.Sqrt)
        recip(rs, rs)
        for c in range(KC):
            stt('dve', hT[:, c, :], xres[:, c, :], gain[:, c:c + 1], rs, ALU.mult, ALU.mult)

    def proj_fm(wu, j, hT, nk=KC):
        ps = psum.next()
        for k in range(nk):
            mm(ps[:], wu[:, k, j * 128:(j + 1) * 128], hT[:, k, :], start=(k == 0), stop=(k == nk - 1))
        return ps

    def residual_proj(name, wsrc, scr, nunits, src_fm):
        for u in range(nunits):
            wu = unit_A(name, wsrc, scr, u)
            for j in range(2):
                m = 2 * u + j
                ps = proj_fm(wu, j, src_fm)
                tt('dve', xres[:, m, :], xres[:, m, :], ps[:], ALU.add)

    def ffn(l, st):
        m1 = ar.mark()
        hT = ar.alloc([KC, T], BF16)
        Abuf = ar.alloc([FC, T], BF16)
        scr = {'sq': [ar.alloc([1, T], BF16)[:, 0, :] for _ in range(2)], 'rstd': ar.alloc([1, T], F32)[:, 0, :]}
        gsb = [ar.alloc([1, T + 2], F32)[:, 0, :] for _ in range(2)]
        acc = [ar.alloc([1, T], F32)[:, 0, :] for _ in range(2)]
        sil = [ar.alloc([1, T], F32)[:, 0, :] for _ in range(2)]
        gain = spf('nffn', l * 16, l * 16 + 16)
        rmsnorm(gain, hT, scr)
        fwb = SP_OFF['fw'] + l * FC * 3
        fbb = SP_OFF['fb'] + l * FC
        ghalo = st['ghalo'][l]
        for i in range(N_GU // 2):
            wg = unit_A(f"gu{l}", w_gu[l], s_gu[l], i)
            wuu = unit_A(f"gu{l}", w_gu[l], s_gu[l], N_GU // 2 + i)
            for j in range(2):
                c = 2 * i + j
                psG = proj_fm(wg, j, hT)
                psU = proj_fm(wuu, j, hT)
                g = gsb[c % 2]
                a = acc[c % 2]
                s_ = sil[c % 2]
                cp('dve', g[:, 0:2], ghalo[:, c, :])
                act(g[:, 2:T + 2], psG[:], ACT.Copy)
                cp('act', ghalo[:, c, :], g[:, T:T + 2])
                ts('dve', a, g[:, 0:T], spk[:, fwb + c * 3:fwb + c * 3 + 1], spk[:, fbb + c:fbb + c + 1], ALU.mult, ALU.add)
                stt('dve', a, g[:, 1:T + 1], spk[:, fwb + c * 3 + 1:fwb + c * 3 + 2], a, ALU.mult, ALU.add)
                stt('dve', a, g[:, 2:T + 2], spk[:, fwb + c * 3 + 2:fwb + c * 3 + 3], a, ALU.mult, ALU.add)
                act(s_, a, ACT.Silu)
                tt('dve', Abuf[:, c, :], psU[:], s_, ALU.mult)
        for m in range(KC):
            ps = psum.next()
            for hf in range(2):
                wd = unit_down(l, m, hf)
                for c2 in range(22):
                    mm(ps[:], wd[:, c2, :], Abuf[:, hf * 22 + c2, :], start=(hf == 0 and c2 == 0), stop=(hf == 1 and c2 == 21))
            tt('dve', xres[:, m, :], xres[:, m, :], ps[:], ALU.add)
        ar.reset(m1)

    mA = ar.mark()
    stA = {}
    stA['R'] = ar.alloc([8, 128], F32)
    stA['Sbf'] = ar.alloc([8, 128], BF16)
    stA['xhalo'] = ar.alloc([8, 3], F32)
    stA['hst'] = ar.alloc([1, 8], F32)[:, 0, :]
    stA['ghalo'] = [ar.alloc([FC, 2], F32), None]
    stA['rot'] = ar.alloc([2, T], F32)
    mA2 = ar.mark()

    def layer0_tile(sq_i, ti):
        st = stA
        m1 = ar.mark()
        stg = [ar.alloc([1, D], F32)[:, 0, :] for _ in range(2)]
        for sub in range(NSUB):
            sg = stg[sub % 2]
            r0 = ti * T + sub * 128
            dma('pool', sg, x_in[sq_i, r0:r0 + 128, :], writes=[sg], chan=f"xl{sub % 2}")
            for c4 in range(4):
                ps = psum.next()
                for cc in range(4):
                    c = c4 * 4 + cc
                    tr(ps[:, cc * 128:(cc + 1) * 128], sg[:, c * 128:(c + 1) * 128], ident)
                cp('act' if c4 % 2 == 0 else 'dve', xres[:, c4 * 4:c4 * 4 + 4, sub * 128:(sub + 1) * 128],
                   ps[:].rearrange("p (a b) -> p a b", a=4))
        ar.reset(m1)
        dma('pool', st['rot'], rot_in[:, :, ti * T:(ti + 1) * T].rearrange("a p t -> p a t"), writes=[st['rot']], chan="rot")
        cosT = st['rot'][:, 0, :]
        sinS = st['rot'][:, 1, :]
        hT = ar.alloc([KC, T], BF16)
        mixed = ar.alloc([KC, T], BF16)
        scr = {'sq': [ar.alloc([1, T], BF16)[:, 0, :] for _ in range(2)], 'rstd': ar.alloc([1, T], F32)[:, 0, :]}
        if stop == 'load':
            ar.reset(m1)
            return
        rmsnorm(spf('nmix', 0, 16), hT, scr)
        if stop == 'norm':
            ar.reset(m1)
            return
        m2 = ar.mark()
        xr = ar.alloc([8, T + 3], F32)
        gy = ar.alloc([8, T], BF16)
        f32s = Rot([ar.alloc([1, T], F32)[:, 0, :] for _ in range(10)])
        xcbs = Rot([ar.alloc([1, T], BF16)[:, 0, :] for _ in range(2)])
        if ti == 0:
            memset('dve', st['xhalo'], 0.0)
            memset('dve', st['hst'], 0.0)
        for n in range(8):
            cp('dve', xr[:, n, 0:3], st['xhalo'][:, n, :])
        for u4 in range(4):
            wx_ = unit_A("ein", w_ein, s_ein, 16 + u4)
            wy_ = unit_A("ein", w_ein, s_ein, 20 + u4)
            for j in range(2):
                n = 2 * u4 + j
                psx = proj_fm(wx_, j, hT)
                act(xr[:, n, 3:T + 3], psx[:], ACT.Copy)
                psy = proj_fm(wy_, j, hT)
                act(gy[:, n, :], psy[:], ACT.Gelu_apprx_tanh)
                cp('act', st['xhalo'][:, n, :], xr[:, n, T:T + 3])
                cwb = SP_OFF['cw'] + n * 4
                xc = f32s.next()
                ts('dve', xc, xr[:, n, 0:T], spk[:, cwb:cwb + 1], spf('cb', n, n + 1), ALU.mult, ALU.add)
                for jj in range(1, 4):
                    stt('dve', xc, xr[:, n, jj:jj + T], spk[:, cwb + jj:cwb + jj + 1], xc, ALU.mult, ALU.add)
                xcb = xcbs.next()
                cp('act', xcb, xc)
                psa = psum.next()
                mm(psa[:], gatesb[:, 0, n, :], xcb)
                psi = psum.next()
                mm(psi[:], gatesb[:, 1, n, :], xcb)
                r_ = f32s.next()
                act(r_, psa[:], ACT.Sigmoid, bias=spf('ba', n, n + 1))
                i_ = f32s.next()
                act(i_, psi[:], ACT.Sigmoid, bias=spf('bx', n, n + 1))
                a_ = f32s.next()
                act(a_, r_, ACT.Exp, scale=clam[:, n:n + 1])
                a2 = f32s.next()
                act(a2, r_, ACT.Exp, scale=clam[:, 8 + n:9 + n])
                ts('dve', a2, a2, -1.0, 1.0, ALU.mult, ALU.add)
                ts('dve', a2, a2, 0.0, None, ALU.max)
                act(a2, a2, ACT.Sqrt)
                tt('dve', i_, i_, xc, ALU.mult)
                tt('dve', i_, i_, a2, ALU.mult)
                P.add('dve', (lambda a_, i_, n: lambda e: e.tensor_tensor_scan(out=a_, data0=a_, data1=i_, initial=st['hst'][:, n:n + 1],
                                                                               op0=ALU.mult, op1=ALU.add))(a_, i_, n),
                      reads=[a_, i_, st['hst'][:, n:n + 1]], writes=[a_])
                cp('act', st['hst'][:, n:n + 1], a_[:, T - 1:T])
                tt('dve', mixed[:, 8 + n, :], a_, gy[:, n, :], ALU.mult)
        ar.reset(m2)
        if stop == 'lru':
            ar.reset(m1)
            return
        qd = ar.alloc([4, T], BF16)
        kd = ar.alloc([4, T], BF16)
        kdt = ar.alloc([NSUB, 512], BF16)
        vt = ar.alloc([NSUB, 512], BF16)
        Gs = ar.alloc([4, T], BF16)
        qsbs = Rot([ar.alloc([1, T], BF16)[:, 0, :] for _ in range(2)])
        f32r = Rot([ar.alloc([1, T], F32)[:, 0, :] for _ in range(6)])
        STs = Rot([ar.alloc([1, T], BF16)[:, 0, :] for _ in range(2)])
        if ti == 0 and not os.environ.get('K_NOMEMSET'):
            memset('dve', st['R'], 0.0)
            memset('dve', st['Sbf'], 0.0)

        def rotary(ps, dst, dec):
            qs = qsbs.next()
            t1 = f32r.next()
            t2 = f32r.next()
            tt('dve', t1, ps[:], cosT, ALU.mult)
            P.add('act', lambda e: e.activation(out=qs, in_=ps[:], func=ACT.Copy), reads=[ps[:], t1], writes=[qs])
            if not os.environ.get('K_NOPERM'):
                ps2 = psum.next()
                mm(ps2[:], permb[:], qs)
                tt('dve', t2, ps2[:], sinS, ALU.mult)
                tt('dve', t1, t1, t2, ALU.add)
            if os.environ.get('K_NOBC'):
                cp('dve', dst, t1)
                return
            tt('dve', dst.rearrange("p (a b) -> p a b", a=4), t1.rearrange("p (a b) -> p a b", a=4),
               dec.unsqueeze(1).to_broadcast([128, 4, 128]), ALU.mult)

        for hb in range(2):
            for uu in range(2):
                wq = unit_A("ein", w_ein, s_ein, hb * 2 + uu)
                for j in range(2):
                    hl = uu * 2 + j
                    h = hb * 4 + hl
                    ps = proj_fm(wq, j, hT)
                    rotary(ps, qd[:, hl, :], csf('qdec', h * 128, (h + 1) * 128))
            if stop == 'ret1':
                ar.reset(m1)
                return
            for uu in range(2):
                wk = unit_A("ein", w_ein, s_ein, 4 + hb * 2 + uu)
                for j in range(2):
                    hl = uu * 2 + j
                    h = hb * 4 + hl
                    ps = proj_fm(wk, j, hT)
                    rotary(ps, kd[:, hl, :], csf('kdec', h * 128, (h + 1) * 128))
                    pst = psum.next()
                    for sub in range(NSUB):
                        mm(pst[:, sub * 128:(sub + 1) * 128], kd[:, hl, sub * 128:(sub + 1) * 128], identb[:])
                    cp('act', kdt[:, :, hl * 128:(hl + 1) * 128], pst[:].rearrange("p (a b) -> p a b", a=4))
            if stop == 'ret2':
                ar.reset(m1)
                return
            for uu in range(2):
                wv = unit_A("ein", w_ein, s_ein, 8 + hb * 2 + uu)
                for sub in range(NSUB):
                    ps = psum.next()
                    for k in range(KC):
                        mm(ps[:, 0:256], hT[:, k, sub * 128:(sub + 1) * 128], wv[:, k, :], start=(k == 0), stop=(k == KC - 1))
                    cp('act' if sub % 2 == 0 else 'dve', vt[:, sub, uu * 256:(uu + 1) * 256], ps[:, 0:256])
            if stop == 'ret3':
                ar.reset(m1)
                return
            for uu in range(2):
                wg_ = unit_A("ein", w_ein, s_ein, 12 + hb * 2 + uu)
                for j in range(2):
                    hl = uu * 2 + j
                    h = hb * 4 + hl
                    ps = proj_fm(wg_, j, hT)
                    sl = f32r.next()
                    act(sl, ps[:], ACT.Silu)
                    ts('dve', Gs[:, hl, :], sl, spf('gn', h, h + 1), None, ALU.mult)
            if stop == 'ret4':
                ar.reset(m1)
                return
            for n in range(NSUB):
                cols = slice(n * 128, (n + 1) * 128)
                psS = psum.next()
                for hl in range(4):
                    mm(psS[:, hl * 128:(hl + 1) * 128], kd[:, hl, cols], qd[:, hl, cols])
                psKV = psum.next()
                for hl in range(4):
                    mm(psKV[:, hl * 128:(hl + 1) * 128], kdt[:, n, hl * 128:(hl + 1) * 128], vt[:, n, hl * 128:(hl + 1) * 128])
                STb = STs.next()
                tt('dve', STb.rearrange("p (a b) -> p a b", a=4), psS[:].rearrange("p (a b) -> p a b", a=4),
                   csf('triu').unsqueeze(1).to_broadcast([128, 4, 128]), ALU.mult)
                psY = psum.next()
                for hl in range(4):
                    h = hb * 4 + hl
                    mm(psY[:, hl * 128:(hl + 1) * 128], vt[:, n, hl * 128:(hl + 1) * 128], STb[:, hl * 128:(hl + 1) * 128], start=True, stop=False)
                    mm(psY[:, hl * 128:(hl + 1) * 128], st['Sbf'][:, h, :], qd[:, hl, cols], start=False, stop=True)
                for hl in range(4):
                    h = hb * 4 + hl
                    stt('dve', st['R'][:, h, :], st['R'][:, h, :], float(CDEC[h]), psKV[:, hl * 128:(hl + 1) * 128], ALU.mult, ALU.add)
                    act(st['Sbf'][:, h, :], st['R'][:, h, :], ACT.Copy, scale=float(CDEC[h]))
                ysq = qsbs.next()
                act(ysq, psY[:], ACT.Square)
                psN = psum.next()
                mm(psN[:], onesb[:], ysq)
                rs = f32r.next()
                ts('dve', rs, psN[:], 1.0 / 128, EPS, ALU.mult, ALU.add)
                act(rs, rs, ACT.Sqrt)
                recip(rs, rs)
                yn = f32r.next()
                tt('dve', yn, psY[:], rs, ALU.mult)
                tt('dve', mixed[:, hb * 4:hb * 4 + 4, cols], yn.rearrange("p (a b) -> p a b", a=4), Gs[:, :, cols], ALU.mult)
        if stop == 'ret':
            ar.reset(m1)
            return
        residual_proj("eout", w_eout, s_eout, N_EOUT, mixed)
        ar.reset(m1)

    def alloc_passB():
        st = {}
        st['Kc'] = ar.alloc([4, S], BF16)
        st['Vc'] = ar.alloc([S // 128, 512], BF16)
        st['ki2'] = ar.alloc([1, S], BF16)[:, 0, :]
        st['ghalo'] = [None, ar.alloc([FC, 2], F32)]
        st['bt'] = [ar.alloc([1, 256], F32)[:, 0, :] for _ in range(2)]
        return st

    def layer1_tile(st, sq_i, ti):
        m1 = ar.mark()
        selT = ar.alloc([S // 128, T], BF16)
        qF = ar.alloc([16, T], BF16)
        m_h = ar.mark()
        hT = ar.alloc([KC, T], BF16)
        attn = hT
        qiF = ar.alloc([8, T], BF16)
        wi = ar.alloc([NSUB, 16], F32)
        scr = {'sq': [ar.alloc([1, T], BF16)[:, 0, :] for _ in range(2)], 'rstd': ar.alloc([1, T], F32)[:, 0, :]}
        rmsnorm(spf('nmix', 16, 32), hT, scr)
        tcols = slice(ti * T, (ti + 1) * T)
        for u in range(8):
            wq = unit_A("oin", w_oin, s_oin, u)
            for j in range(2):
                h = 2 * u + j
                ps = proj_fm(wq, j, hT)
                act(qF[:, h, :], ps[:], ACT.Copy, scale=128.0 ** -0.5)
        for u in range(2):
            wk = unit_A("oin", w_oin, s_oin, 8 + u)
            for j in range(2):
                kvh = 2 * u + j
                ps = proj_fm(wk, j, hT)
                cp('dve', st['Kc'][:, kvh, tcols], ps[:])
        for u in range(2):
            wv = unit_A("oin", w_oin, s_oin, 10 + u)
            for sub in range(NSUB):
                ps = psum.next()
                for k in range(KC):
                    mm(ps[:, 0:256], hT[:, k, sub * 128:(sub + 1) * 128], wv[:, k, :], start=(k == 0), stop=(k == KC - 1))
                cp('act' if sub % 2 == 0 else 'dve', st['Vc'][:, ti * NSUB + sub, u * 256:(u + 1) * 256], ps[:, 0:256])
        for u in range(4):
            wqi = unit_A("oin", w_oin, s_oin, 12 + u)
            for j in range(2):
                cq = 2 * u + j
                ps = proj_fm(wqi, j, hT)
                cp('act', qiF[:, cq, :], ps[:])
        wl = unit_oin_last()
        ps = proj_fm(wl, 0, hT)
        cp('dve', st['ki2'][:, tcols], ps[:])
        for sub in range(NSUB):
            ps = psum.next()
            for k in range(KC):
                mm(ps[:, 0:16], hT[:, k, sub * 128:(sub + 1) * 128], wl[:, k, 128:144], start=(k == 0), stop=(k == KC - 1))
            act(wi[:, sub, :], ps[:, 0:16], ACT.Copy, scale=1.0 / 32.0)
        m2 = ar.mark()
        Lt = T * (ti + 1)
        na = Lt // 128
        score = ar.alloc([1, S], F32)[:, 0, :]
        sel = ar.alloc([1, S], BF16)[:, 0, :]
        junk = ar.alloc([1, S], BF16)[:, 0, :]
        Rb = Rot([ar.alloc([1, T], F32)[:, 0, :] for _ in range(3)])
        sm = ar.alloc([1, 8 + NIT], F32)[:, 0, :]
        for sub in range(NSUB):
            gt = ti * NSUB + sub
            scol = slice(sub * 128, (sub + 1) * 128)
            if gt < 2:
                for a in range(na):
                    dst = selT[:, a, scol]
                    if a < gt:
                        cp('act', dst, onesb[:])
                    elif a == gt:
                        cp('act', dst, trib[:])
                    else:
                        memset('dve', dst, 0.0)
                continue
            L = 128 * (gt + 1)
            nblk = (L + 511) // 512
            for sbk in range(nblk):
                w = min(512, L - 512 * sbk)
                blk = slice(sbk * 512, sbk * 512 + w)
                for h in range(16):
                    cq = h // 2
                    rows = slice((h % 2) * 64, (h % 2) * 64 + 64)
                    ps = psum.next()
                    mm(ps[:, 0:w], qiF[rows, cq, scol], st['ki2'][rows, blk])
                    rb = Rb.next()
                    act(rb[:, 0:w], ps[:, 0:w], ACT.Relu)
                    if h == 0:
                        ts('dve', score[:, blk], rb[:, 0:w], wi[:, sub, 0:1], None, ALU.mult)
                    else:
                        stt('dve', score[:, blk], rb[:, 0:w], wi[:, sub, h:h + 1], score[:, blk], ALU.mult, ALU.add)
            ts('dve', junk[:, 0:L], score[:, 0:L], 1.0, None, ALU.mult, ALU.max, accum=sm[:, 0:1])
            ts('dve', junk[:, 0:L], score[:, 0:L], 1.0, None, ALU.mult, ALU.min, accum=sm[:, 1:2])
            tt('dve', score[:, L - 128:L], score[:, L - 128:L], csf('negm'), ALU.add)
            if L < Lt:
                memset('dve', score[:, L:Lt], NEG)
            tt('dve', sm[:, 5:6], sm[:, 0:1], sm[:, 1:2], ALU.subtract)
            ts('dve', sm[:, 8:8 + NIT], csf('pow2'), sm[:, 5:6], None, ALU.mult)
            for k in range(NIT):
                tt('dve', sm[:, 2:3], sm[:, 1:2], sm[:, 8 + k:9 + k], ALU.add)
                ts('dve', junk[:, 0:L], score[:, 0:L], sm[:, 2:3], None, ALU.is_ge, ALU.add, accum=sm[:, 3:4])
                stt('dve', sm[:, 4:5], sm[:, 3:4], 256.0, sm[:, 8 + k:9 + k], ALU.is_ge, ALU.mult)
                tt('dve', sm[:, 1:2], sm[:, 1:2], sm[:, 4:5], ALU.add)
            ts('dve', sel[:, 0:Lt], score[:, 0:Lt], sm[:, 1:2], None, ALU.is_ge)
            for a0 in range(0, na, 4):
                n4 = min(4, na - a0)
                pst = psum.next()
                for a in range(n4):
                    mm(pst[:, a * 128:(a + 1) * 128], sel[:, (a0 + a) * 128:(a0 + a + 1) * 128], identb[:])
                cp('act', selT[:, a0:a0 + n4, scol], pst[:, 0:n4 * 128].rearrange("p (a b) -> p a b", a=n4))
        ar.reset(m2)
        Eb = Rot([ar.alloc([1, T], BF16)[:, 0, :] for _ in range(3)])
        Pm = Rot([ar.alloc([1, T], BF16)[:, 0, :] for _ in range(3)])
        tf = Rot([ar.alloc([1, 256], F32)[:, 0, :] for _ in range(2)])
        rz = Rot([ar.alloc([1, T], F32)[:, 0, :] for _ in range(2)])
        acc_banks = Rot(psb[0:4])
        tmp_banks = Rot(psb[4:8])
        for hq in range(16):
            kvh = hq // 4
            btb = st['bt'][hq % 2]
            dma('pool', btb, bt_in[hq], writes=[btb], chan=f"bt{hq % 2}")
            psO = acc_banks.next()
            psZ = acc_banks.next()
            for a in range(na):
                sa = a - ti * NSUB
                c0 = max(0, sa) * 128
                psL = tmp_banks.next()
                mm(psL[:, c0:T], st['Kc'][:, kvh, a * 128:(a + 1) * 128], qF[:, hq, c0:T])
                eb = Eb.next()
                n1 = c0
                if sa >= -1:
                    n0 = max(0, sa) * 128
                    n1 = min(NSUB, sa + 2) * 128
                    tfb = tf.next()
                    tt('dve', tfb[:, 0:n1 - n0], psL[:, n0:n1], btb[:, n0 - 128 * sa:n1 - 128 * sa], ALU.add)
                    act(eb[:, n0:n1], tfb[:, 0:n1 - n0], ACT.Exp)
                if n1 < T:
                    act(eb[:, n1:T], psL[:, n1:T], ACT.Exp, bias=csf('c31', hq, hq + 1))
                pm = Pm.next()
                tt('dve', pm[:, c0:T], eb[:, c0:T], selT[:, a, c0:T], ALU.mult)
                mm(psO[:, c0:T], st['Vc'][:, a, kvh * 128:(kvh + 1) * 128], pm[:, c0:T], start=(a == 0), stop=(a == na - 1))
                mm(psZ[:, c0:T], onesb[:], pm[:, c0:T], start=(a == 0), stop=(a == na - 1))
            r = rz.next()
            recip(r, psZ[:])
            tt('dve', attn[:, hq, :], psO[:], r, ALU.mult)
        residual_proj("oout", w_oout, s_oout, N_OOUT, attn)
        ar.reset(m1)

    def final_tile(sq_i, ti):
        m1 = ar.mark()
        scr = {'sq': [ar.alloc([1, T], BF16)[:, 0, :] for _ in range(2)], 'rstd': ar.alloc([1, T], F32)[:, 0, :]}
        ostg = [ar.alloc([1, D], F32)[:, 0, :] for _ in range(2)]
        ss = psum.next()
        for c in range(KC):
            sq = scr['sq'][c % 2]
            act(sq, xres[:, c, :], ACT.Square)
            mm(ss[:], onesb[:], sq, start=(c == 0), stop=(c == KC - 1))
        rs = scr['rstd']
        ts('dve', rs, ss[:], 1.0 / D, EPS, ALU.mult, ALU.add)
        act(rs, rs, ACT.Sqrt)
        recip(rs, rs)
        for c in range(KC):
            stt('dve', xres[:, c, :], xres[:, c, :], spf('fnorm', c, c + 1), rs, ALU.mult, ALU.mult)
        chs = []
        for sub in range(NSUB):
            og = ostg[sub % 2]
            for c4 in range(4):
                ps = psum.next()
                for cc in range(4):
                    c = c4 * 4 + cc
                    tr(ps[:, cc * 128:(cc + 1) * 128], xres[:, c, sub * 128:(sub + 1) * 128], ident)
                cp('act' if c4 % 2 == 0 else 'dve', og[:, c4 * 512:(c4 + 1) * 512], ps[:])
            r0 = ti * T + sub * 128
            chs.append(dma('pool', out[sq_i, r0:r0 + 128, :], og, reads=[og], chan=f"os{sub % 2}"))
        ar.reset(m1)
        return chs

    final_chans = set()
    for sq_i in range(nseq):
        ar.reset(mA2)
        for ti in range(NTL):
            if ti == 0:
                memset('dve', stA['ghalo'][0], 0.0)
            layer0_tile(sq_i, ti)
            if stop is None or stop == 'ffn':
                ffn(0, stA)
            key = f"x2:{sq_i}:{ti}"
            ch = dma('pool', x2s[sq_i, ti], xres[:].rearrange("p a b -> p (a b)"), reads=[xres[:]], writes=[key], chan="x2st")
            if debug_stage == 'A':
                final_chans.add(ch)
        if debug_stage == 'A':
            continue
        ar.reset(mA)
        stB = alloc_passB()
        for ti in range(NT):
            key = f"x2:{sq_i}:{ti}"
            dma('pool', xres[:].rearrange("p a b -> p (a b)"), x2s[sq_i, ti], reads=[key], writes=[xres[:]], chan="x2ld")
            if ti == 0:
                memset('dve', stB['ghalo'][1], 0.0)
            layer1_tile(stB, sq_i, ti)
            ffn(1, stB)
            for ch in final_tile(sq_i, ti):
                final_chans.add(ch)
        ar.reset(mA)
        ar.reset(mA2)
    P.emit(final_chans=sorted(final_chans))
    return nc, P


def _fm(v, nchunk):
    return np.ascontiguousarray(np.asarray(v, np.float32).reshape(nchunk, 128).T)


def _t5_bucket(n):
    n = np.maximum(n, 0)
    nf = np.maximum(n, 16).astype(np.float32)
    large = 16 + (np.log(nf / np.float32(16)) / np.float32(math.log(128 / 16)) * np.float32(16)).astype(np.int32)
    large = np.minimum(large, 31)
    return np.where(n < 16, n, large)


def host_prep(inp):
    f = np.float32
    sp = np.zeros((128, SP_W), f)

    def put(name, arr):
        arr = np.asarray(arr, f).reshape(128, -1)
        sp[:, SP_OFF[name]:SP_OFF[name] + arr.shape[1]] = arr

    put('nmix', np.stack([_fm(inp['norm_mix'][l], 16) for l in range(2)], 1))
    put('nffn', np.stack([_fm(inp['norm_ffn'][l], 16) for l in range(2)], 1))
    put('fnorm', _fm(inp['final_norm'], 16))
    put('gn', _fm(inp['e_ret_gn'][0], 8))
    put('cw', np.asarray(inp['e_conv_w'][0], f).reshape(4, 8, 128).transpose(2, 1, 0))
    put('cb', _fm(inp['e_conv_b'][0], 8))
    put('ba', _fm(inp['e_gate_a_b'][0], 8))
    put('bx', _fm(inp['e_gate_x_b'][0], 8))
    put('lam', _fm(inp['e_lambda'][0], 8))
    put('fw', np.asarray(inp['ffn_conv_w'], f).reshape(2, 3, FC, 128).transpose(3, 0, 2, 1))
    put('fb', np.asarray(inp['ffn_conv_b'], f).reshape(2, FC, 128).transpose(2, 0, 1))

    cs = np.zeros((128, CS_W), f)

    def putc(name, arr):
        arr = np.asarray(arr, f).reshape(128, -1)
        cs[:, CS_OFF[name]:CS_OFF[name] + arr.shape[1]] = arr

    idx = np.arange(128)
    putc('ident', np.eye(128, dtype=f))
    putc('triu', (idx[None, :] >= idx[:, None]).astype(f))
    perm = np.zeros((128, 128), f)
    perm[(idx + 64) % 128, idx] = 1.0
    putc('perm', perm)
    putc('negm', np.where(idx[None, :] > idx[:, None], f(NEG), f(0)))
    g64 = np.array(GAMMA, np.float64)
    i64 = np.arange(128, dtype=np.float64)
    qdec = g64[:, None] ** (i64[None, :] + 1.0)
    kdec = g64[:, None] ** (-(i64[None, :] + 1.0)) * (128.0 ** -0.5)
    putc('qdec', np.broadcast_to(qdec.reshape(1, 1024), (128, 1024)))
    putc('kdec', np.broadcast_to(kdec.reshape(1, 1024), (128, 1024)))
    putc('c31', np.broadcast_to(np.asarray(inp['rel_bias'], f)[31][None, :], (128, 16)))
    putc('pow2', np.broadcast_to((2.0 ** (-(np.arange(NIT, dtype=np.float64) + 1.0)))[None, :], (128, NIT)))

    gates = np.stack([np.asarray(inp['e_gate_a_w'][0], f).transpose(1, 0, 2),
                      np.asarray(inp['e_gate_x_w'][0], f).transpose(1, 0, 2)], 1)
    half = 64
    freqs = (f(10000.0) ** (-np.arange(half, dtype=f) / f(half))).astype(f)
    ang = (np.arange(S, dtype=f)[:, None] * freqs[None, :]).astype(f)
    cos = np.cos(ang).astype(f).T
    sin = np.sin(ang).astype(f).T
    rot = np.stack([np.concatenate([cos, cos], 0), np.concatenate([-sin, sin], 0)], 0)
    rel = idx[None, :256 - 128 + 128] if False else (np.arange(256)[None, :] - idx[:, None])
    bidx = _t5_bucket(rel)
    bt = np.ascontiguousarray(np.asarray(inp['rel_bias'], f)[bidx].transpose(2, 0, 1))
    return dict(smallp=sp, consts=cs, gates=np.ascontiguousarray(gates), rot=np.ascontiguousarray(rot), bt=bt)


_CACHE = {}


def kernel(**inputs):
    n_cores = 8
    inp = {k: np.asarray(v) for k, v in inputs.items()}
    hp = host_prep(inp)
    shared = dict(hp)
    shared['e_w_in'] = np.ascontiguousarray(inp['e_w_in'][0], dtype=np.float32)
    shared['e_w_out'] = np.ascontiguousarray(inp['e_w_out'][0], dtype=np.float32)
    shared['o_w_in'] = np.ascontiguousarray(inp['o_w_in'][0], dtype=np.float32)
    shared['o_w_out'] = np.ascontiguousarray(inp['o_w_out'][0], dtype=np.float32)
    shared['ffn_w_gu'] = np.ascontiguousarray(inp['ffn_w_gu'], dtype=np.float32)
    shared['ffn_w_down'] = np.ascontiguousarray(inp['ffn_w_down'], dtype=np.float32)
    x = np.ascontiguousarray(inp['x'], dtype=np.float32)
    if 'nc' not in _CACHE:
        _CACHE['nc'] = build_program()[0]
    nc = _CACHE['nc']
    in_maps = []
    for c in range(n_cores):
        m = dict(shared)
        m['x'] = x[c * NSEQ:(c + 1) * NSEQ]
        in_maps.append(m)
    res = run_bass_kernel_spmd(nc, in_maps, core_ids=list(range(n_cores)))
    return np.concatenate([r['out'] for r in res.results], axis=0)
```

```python
import math
import os
import numpy as np
import concourse.bass as bass
import concourse.mybir as mybir
from concourse.bass_utils import run_bass_kernel_spmd

ACT = mybir.ActivationFunctionType
ALU = mybir.AluOpType
F32 = mybir.dt.float32
BF16 = mybir.dt.bfloat16

CELL = 256
STRICT = bool(os.environ.get('K_STRICT'))
RAW_WINDOW = 2
EPOCH = 30000
ENGS = ('pe', 'act', 'dve', 'pool', 'sp')


class Op:
    __slots__ = ('eng', 'fn', 'deps', 'signal', 'sig', 'is_dma', 'chan', 'dval', 'idx')

    def __init__(self, eng, fn, is_dma=False, chan=None):
        self.eng = eng
        self.fn = fn
        self.deps = []
        self.signal = False
        self.sig = 0
        self.is_dma = is_dma
        self.chan = chan
        self.dval = 0
        self.idx = 0


class Prog:
    def __init__(self, nc):
        self.nc = nc
        self.ops = {e: [] for e in ENGS}
        self.cells = {}
        self.chan_last = {}
        self.chan_cnt = {}
        self.n_ops = 0

    def _cells(self, ap):
        if isinstance(ap, str):
            return [('k', ap)]
        t = ap.tensor
        if 'DRam' in type(t).__name__:
            return []
        esz = mybir.dt.size(ap.dtype)
        pat = ap.ap
        pstep = pat[0][0]
        off = ap.offset
        if pstep > 0:
            off = off % pstep
        lo = off
        hi = off
        for (st, cnt) in pat[1:]:
            if st >= 0:
                hi += st * (cnt - 1)
            else:
                lo += st * (cnt - 1)
        lo_b = lo * esz
        hi_b = (hi + 1) * esz
        name = t.name
        if name.startswith('psb'):
            return [(name, 0)]
        return [(name, c) for c in range(lo_b // CELL, (hi_b - 1) // CELL + 1)]

    def add(self, eng, fn, reads=(), writes=(), chan=None):
        is_dma = chan is not None
        op = Op(eng, fn, is_dma, chan)
        self.n_ops += 1
        deps = {}

        def dep(d):
            if d is None or d is op:
                return
            if d.is_dma:
                deps[('dma', id(d))] = d
            else:
                k = ('c', d.eng)
                o = deps.get(k)
                if o is None or d.idx > o.idx:
                    deps[k] = d

        rcells = []
        for r in reads:
            if r is not None and not isinstance(r, (int, float)):
                rcells.extend(self._cells(r))
        wcells = []
        for w in writes:
            wcells.extend(self._cells(w))
        cells = self.cells
        nxt = len(self.ops[eng])
        for c in rcells:
            rec = cells.get(c)
            if rec is not None:
                w = rec[0]
                if w is not None and (STRICT or w.is_dma or is_dma or w.eng != eng or nxt - w.idx <= RAW_WINDOW):
                    dep(w)
                if c[0].startswith('psb'):
                    for e2, r in rec[1].items():
                        if e2 != eng:
                            dep(r)
        for c in wcells:
            rec = cells.get(c)
            if rec is not None:
                w = rec[0]
                if w is not None and (w.is_dma or is_dma or w.eng != eng or (STRICT and eng != 'pe')):
                    dep(w)
                for e2, r in rec[1].items():
                    if e2 != eng or is_dma or (STRICT and eng != 'pe'):
                        dep(r)
                for r in rec[2]:
                    dep(r)
        if is_dma:
            prev = self.chan_last.get(chan)
            if prev is not None:
                dep(prev)
            self.chan_last[chan] = op
            n = self.chan_cnt.get(chan, 0) + 1
            self.chan_cnt[chan] = n
            op.dval = 16 * n
        lst = self.ops[eng]
        op.idx = len(lst)
        lst.append(op)
        for d in deps.values():
            if not d.is_dma:
                d.signal = True
            op.deps.append(d)
        for c in wcells:
            cells[c] = [op, {}, []]
        for c in rcells:
            rec = cells.get(c)
            if rec is None:
                rec = [None, {}, []]
                cells[c] = rec
            if is_dma:
                rec[2].append(op)
            else:
                rec[1][eng] = op
        return op

    def emit(self, final_chans=()):
        nc = self.nc
        from contextlib import ExitStack
        with ExitStack() as es:
            nsig = {}
            for e in ENGS:
                n = 0
                for op in self.ops[e]:
                    if op.signal and not op.is_dma:
                        n += 1
                        op.sig = n
                nsig[e] = n
            sems = {}
            for e in ENGS:
                ne = max((nsig[e] + EPOCH - 1) // EPOCH, 1)
                sems[e] = [es.enter_context(nc.semaphore(f'c_{e}_{i}')) for i in range(ne)]
            chans = {}
            for ch in self.chan_cnt:
                chans[ch] = es.enter_context(nc.semaphore(f'd_{ch}'))
            block = es.enter_context(nc.Block())

            def make(e):
                def body(eng):
                    waited_c = {}
                    waited_d = {}
                    for op in self.ops[e]:
                        for d in op.deps:
                            if d.is_dma:
                                if waited_d.get(d.chan, 0) < d.dval:
                                    eng.wait_ge(chans[d.chan], d.dval)
                                    waited_d[d.chan] = d.dval
                            else:
                                if waited_c.get(d.eng, 0) < d.sig:
                                    ep = (d.sig - 1) // EPOCH
                                    eng.wait_ge(sems[d.eng][ep], (d.sig - 1) % EPOCH + 1)
                                    waited_c[d.eng] = d.sig
                        ins = op.fn(eng)
                        if op.is_dma:
                            ins.then_inc(chans[op.chan], 16)
                        elif op.signal:
                            ep = (op.sig - 1) // EPOCH
                            ins.then_inc(sems[e][ep], 1)
                    if e == 'sp':
                        for ch in final_chans:
                            eng.wait_ge(chans[ch], 16 * self.chan_cnt[ch])
                return body

            block.tensor(make('pe'))
            block.scalar(make('act'))
            block.vector(make('dve'))
            block.gpsimd(make('pool'))
            block.sync(make('sp'))


D = 2048
S = 2048
NSEQ = 2
T = 512
NT = S // T
NSUB = T // 128
KC = D // 128
E_IN = 6144
O_IN = 4176
DFF = 5632
FC = DFF // 128
EPS = 1e-6
NSLOT = 4
SLOT = 4096
NIT = 16
NEG = -1e30

SP_FIELDS = [('nmix', 32), ('nffn', 32), ('fnorm', 16), ('gn', 8), ('cw', 32), ('cb', 8), ('ba', 8),
             ('bx', 8), ('lam', 8), ('fw', 2 * FC * 3), ('fb', 2 * FC)]
SP_OFF = {}
_o = 0
for _n, _w in SP_FIELDS:
    SP_OFF[_n] = _o
    _o += _w
SP_W = _o
CS_FIELDS = [('ident', 128), ('triu', 128), ('perm', 128), ('negm', 128), ('qdec', 1024), ('kdec', 1024),
             ('c31', 16), ('pow2', NIT)]
CS_OFF = {}
_o = 0
for _n, _w in CS_FIELDS:
    CS_OFF[_n] = _o
    _o += _w
CS_W = _o

GAMMA = [1.0 - 2.0 ** (-5.0 - h) for h in range(8)]
CDEC = [g ** 128 for g in GAMMA]

N_EIN = E_IN // 256
N_EOUT = 8
N_GU = (2 * DFF) // 256
N_DOWN = 32
N_OIN = 17
N_OOUT = 8


def build_program(nseq=NSEQ, debug_stage=None, stop=None, ntiles=None):
    NTL = NT if ntiles is None else ntiles
    nc = bass.Bass("TRN2", target_bir_lowering=False)
    P = Prog(nc)

    def din(name, shape, dt=F32):
        return nc.dram_tensor(name, list(shape), dt, kind="ExternalInput").ap()

    x_in = din("x", [nseq, S, D])
    w_ein = din("e_w_in", [D, E_IN])
    w_eout = din("e_w_out", [D, D])
    w_oin = din("o_w_in", [D, O_IN])
    w_oout = din("o_w_out", [D, D])
    w_gu = din("ffn_w_gu", [2, D, 2 * DFF])
    w_dn = din("ffn_w_down", [2, DFF, D])
    sp_in = din("smallp", [128, SP_W])
    cs_in = din("consts", [128, CS_W])
    gates_in = din("gates", [128, 2, 8, 128])
    rot_in = din("rot", [2, 128, S])
    bt_in = din("bt", [16, 128, 256])
    out = nc.dram_tensor("out", [nseq, S, D], F32, kind="ExternalOutput").ap()

    def dscr(name, shape, dt):
        return nc.dram_tensor(name, list(shape), dt, kind="Internal").ap()

    s_ein = dscr("s_ein", [N_EIN, 128, SLOT], BF16)
    s_eout = dscr("s_eout", [N_EOUT, 128, SLOT], BF16)
    s_oin = dscr("s_oin", [N_OIN, 128, SLOT], BF16)
    s_oout = dscr("s_oout", [N_OOUT, 128, SLOT], BF16)
    s_gu = dscr("s_gu", [2, N_GU, 128, SLOT], BF16)
    s_dn = dscr("s_dn", [2, N_DOWN, 128, 22 * 128], BF16)
    if debug_stage == 'A':
        x2s = nc.dram_tensor("x2s", [nseq, NT, 128, KC * T], F32, kind="ExternalOutput").ap()
    else:
        x2s = dscr("x2s", [nseq, NT, 128, KC * T], F32)

    sb = nc.alloc_sbuf_tensor
    ring = sb("ring", [128, NSLOT, SLOT], BF16)
    xres = sb("xres", [128, KC, T], F32)
    spk = sb("spk", [128, SP_W], F32)
    cst = sb("cst", [128, CS_W], F32)
    identb = sb("identb", [128, 128], BF16)
    permb = sb("permb", [128, 128], BF16)
    onesb = sb("onesb", [128, 128], BF16)
    trib = sb("trib", [128, 128], BF16)
    gatesb = sb("gatesb", [128, 2, 8, 128], BF16)
    clam = sb("clam", [128, 16], F32)
    ARENA = 122 * 1024
    arena = sb("arena", [128, ARENA // 2], BF16)
    psb = [nc.alloc_psum_tensor(f"psb{i}", [128, 512], F32) for i in range(8)]

    def spf(name, lo=0, hi=None):
        o = SP_OFF[name]
        w = dict(SP_FIELDS)[name]
        hi = w if hi is None else hi
        return spk[:, o + lo:o + hi]

    def csf(name, lo=0, hi=None):
        o = CS_OFF[name]
        w = dict(CS_FIELDS)[name]
        hi = w if hi is None else hi
        return cst[:, o + lo:o + hi]

    ident = csf('ident')

    class Arena:
        def __init__(self):
            self.top = 0

        def alloc(self, shape, dt):
            n = 1
            for s_ in shape:
                n *= s_
            esz = mybir.dt.size(dt)
            nbytes = ((n * esz + 255) // 256) * 256
            lo = self.top
            self.top += nbytes
            assert self.top <= ARENA, f"arena overflow {self.top}"
            v = arena[:, lo // 2:(lo + n * esz) // 2]
            if dt != BF16:
                v = v.bitcast(dt)
            if len(shape) == 2:
                return v.rearrange("p (a b) -> p a b", a=shape[0])
            if len(shape) == 3:
                return v.rearrange("p (a b c) -> p a b c", a=shape[0], b=shape[1])
            return v

        def mark(self):
            return self.top

        def reset(self, m):
            self.top = m

    ar = Arena()

    class Rot:
        def __init__(self, items):
            self.items = items
            self.i = 0

        def next(self):
            v = self.items[self.i % len(self.items)]
            self.i += 1
            return v

    psum = Rot(psb[0:7])
    ssb = psb[7]

    def mm(o, lhsT, rhs, start=True, stop=True):
        P.add('pe', lambda e: e.matmul(o, lhsT=lhsT, rhs=rhs, start=start, stop=stop), reads=[lhsT, rhs], writes=[o])

    def tr(o, in_, idn):
        P.add('pe', lambda e: e.transpose(o, in_, idn), reads=[in_, idn], writes=[o])

    def act(o, in_, func, bias=None, scale=None):
        kw = {}
        if bias is not None:
            kw['bias'] = bias
        if scale is not None:
            kw['scale'] = scale
        P.add('act', lambda e: e.activation(out=o, in_=in_, func=func, **kw), reads=[in_, bias, scale], writes=[o])

    def tt(eng, o, a, b, op):
        P.add(eng, lambda e: e.tensor_tensor(out=o, in0=a, in1=b, op=op), reads=[a, b], writes=[o])

    def ts(eng, o, a, s1, s2, op0, op1=None, accum=None):
        kw = {}
        if op1 is not None:
            kw['op1'] = op1
        if accum is not None:
            kw['accum_out'] = accum
        wr = [o] + ([accum] if accum is not None else [])
        P.add(eng, lambda e: e.tensor_scalar(out=o, in0=a, scalar1=s1, scalar2=s2, op0=op0, **kw), reads=[a, s1, s2], writes=wr)

    def stt(eng, o, a, sc, b, op0, op1):
        P.add(eng, lambda e: e.scalar_tensor_tensor(out=o, in0=a, scalar=sc, in1=b, op0=op0, op1=op1), reads=[a, sc, b], writes=[o])

    def cp(eng, o, a):
        if eng == 'act':
            act(o, a, ACT.Copy)
        else:
            P.add(eng, lambda e: e.tensor_copy(out=o, in_=a), reads=[a], writes=[o])

    def memset(eng, o, val):
        P.add(eng, lambda e: e.memset(o, val), writes=[o])

    def recip(o, a):
        P.add('dve', lambda e: e.reciprocal(out=o, in_=a), reads=[a], writes=[o])

    dma_rr = {'n': 0}

    def dma(q, o, i, reads=(), writes=(), chan=None):
        if chan is None:
            chan = f"g{dma_rr['n'] % 6}"
            dma_rr['n'] += 1
        P.add(q, lambda e: e.dma_start(out=o, in_=i), reads=list(reads), writes=list(writes), chan=chan)
        return chan

    dma('pool', spk[:], sp_in, writes=[spk[:]])
    dma('pool', cst[:], cs_in, writes=[cst[:]])
    m0 = ar.mark()
    gst = ar.alloc([2 * 8, 128], F32)
    dma('pool', gst, gates_in.rearrange("p a n e -> p (a n) e"), writes=[gst])
    cp('act', identb[:], ident)
    cp('dve', permb[:], csf('perm'))
    cp('dve', trib[:], csf('triu'))
    memset('dve', onesb[:], 1.0)
    cp('act', gatesb[:].rearrange("p a n e -> p (a n) e"), gst)
    act(clam[:, 0:8], spf('lam'), ACT.Exp, scale=-1.0)
    act(clam[:, 0:8], clam[:, 0:8], ACT.Ln, bias=1.0)
    ts('dve', clam[:, 8:16], clam[:, 0:8], -16.0, None, ALU.mult)
    ts('dve', clam[:, 0:8], clam[:, 0:8], -8.0, None, ALU.mult)
    ar.reset(m0)

    cast_done = set()
    wcnt = {'n': 0, 'c': 0}

    def cast_unit(key, dst, src):
        if key in cast_done:
            return
        cast_done.add(key)
        ch = f"cast{wcnt['c'] % 8}"
        wcnt['c'] += 1
        P.add('pool', lambda e: e.dma_start(out=dst, in_=src), writes=[key], chan=ch)

    def load_unit(key, src_scr, shape3):
        slot = wcnt['n'] % NSLOT
        wcnt['n'] += 1
        a, b = shape3
        dst = ring[:, slot, 0:a * b]
        P.add('sp', lambda e: e.dma_start(out=dst, in_=src_scr), reads=[key], writes=[dst], chan=f"w{slot}")
        return dst.rearrange("p (a b) -> p a b", a=a)

    def unit_A(name, wsrc, scr, u):
        key = f"{name}:{u}"
        src = wsrc[:, u * 256:(u + 1) * 256].rearrange("(k p) j -> p k j", p=128)
        cast_unit(key, scr[u].rearrange("p (k j) -> p k j", k=KC), src)
        return load_unit(key, scr[u], (KC, 256))

    def unit_oin_last():
        key = "oin:16"
        if key not in cast_done:
            cast_done.add(key)
            dstc = s_oin[16].rearrange("p (k j) -> p k j", k=KC)
            ki_src = w_oin[:, 4096:4160].rearrange("(k p) j -> p k j", p=128)
            wi_src = w_oin[:, 4160:4176].rearrange("(k p) j -> p k j", p=128)
            P.add('pool', lambda e: e.dma_start(out=dstc[:, :, 0:64], in_=ki_src), writes=[key + 'a'], chan="castx0")
            P.add('pool', lambda e: e.dma_start(out=dstc[:, :, 64:128], in_=ki_src), writes=[key + 'b'], chan="castx1")
            P.add('pool', lambda e: e.dma_start(out=dstc[:, :, 128:144], in_=wi_src), writes=[key + 'c'], chan="castx2")
        slot = wcnt['n'] % NSLOT
        wcnt['n'] += 1
        dst = ring[:, slot, :].rearrange("p (a b) -> p a b", a=KC)
        srcv = s_oin[16].rearrange("p (k j) -> p k j", k=KC)
        P.add('sp', lambda e: e.dma_start(out=dst[:, :, 0:144], in_=srcv[:, :, 0:144]), reads=[key + 'a', key + 'b', key + 'c'],
              writes=[dst[:, :, 0:144]], chan=f"w{slot}")
        return dst

    def unit_down(l, m, hf):
        key = f"dn{l}:{m}:{hf}"
        src = w_dn[l, hf * 22 * 128:(hf + 1) * 22 * 128, m * 128:(m + 1) * 128].rearrange("(c p) j -> p c j", p=128)
        cast_unit(key, s_dn[l, m * 2 + hf].rearrange("p (c j) -> p c j", c=22), src)
        return load_unit(key, s_dn[l, m * 2 + hf], (22, 128))

    def rmsnorm(gain, hT, scr, ss_ready=False):
        ss = ssb
        if not ss_ready:
            for c in range(KC):
                sq = scr['sq'][c % 2]
                act(sq, xres[:, c, :], ACT.Square)
                mm(ss[:], onesb[:], sq, start=(c == 0), stop=(c == KC - 1))
        rs = scr['rstd']
        ts('dve', rs, ss[:], 1.0 / D, EPS, ALU.mult, ALU.add)
        act(rs, rs, ACT.Sqrt)
        recip(rs, rs)
        for c in range(KC):
            stt('dve', hT[:, c, :], xres[:, c, :], gain[:, c:c + 1], rs, ALU.mult, ALU.mult)

    def proj_fm(wu, j, hT, nk=KC):
        ps = psum.next()
        for k in range(nk):
            mm(ps[:], wu[:, k, j * 128:(j + 1) * 128], hT[:, k, :], start=(k == 0), stop=(k == nk - 1))
        return ps

    class SSFuse:
        def __init__(self):
            self.m0 = ar.mark()
            self.sq = Rot([ar.alloc([1, T], BF16)[:, 0, :] for _ in range(4)])
            self.pend = []

        def chunk_done(self, m):
            sq = self.sq.next()
            act(sq, xres[:, m, :], ACT.Square)
            self.pend.append((m, sq))
            if len(self.pend) > 2:
                self.flush1()

        def flush1(self):
            m, sq = self.pend.pop(0)
            mm(ssb[:], onesb[:], sq, start=(m == 0), stop=(m == KC - 1))

        def finish(self):
            while self.pend:
                self.flush1()
            ar.reset(self.m0)

    def residual_proj(name, wsrc, scr, nunits, src_fm, fuse=True):
        sf = SSFuse() if fuse else None
        for u in range(nunits):
            wu = unit_A(name, wsrc, scr, u)
            for j in range(2):
                m = 2 * u + j
                ps = proj_fm(wu, j, src_fm)
                tt('dve', xres[:, m, :], xres[:, m, :], ps[:], ALU.add)
                if sf:
                    sf.chunk_done(m)
        if sf:
            sf.finish()

    def ffn(l, st, fuse_next=False):
        m1 = ar.mark()
        hT = ar.alloc([KC, T], BF16)
        Abuf = ar.alloc([FC, T], BF16)
        scr = {'sq': [ar.alloc([1, T], BF16)[:, 0, :] for _ in range(2)], 'rstd': ar.alloc([1, T], F32)[:, 0, :]}
        gsb = [ar.alloc([1, T + 2], F32)[:, 0, :] for _ in range(2)]
        acc = [ar.alloc([1, T], F32)[:, 0, :] for _ in range(2)]
        sil = [ar.alloc([1, T], F32)[:, 0, :] for _ in range(2)]
        gain = spf('nffn', l * 16, l * 16 + 16)
        rmsnorm(gain, hT, scr, ss_ready=True)
        fwb = SP_OFF['fw'] + l * FC * 3
        fbb = SP_OFF['fb'] + l * FC
        ghalo = st['ghalo'][l]
        for i in range(N_GU // 2):
            wg = unit_A(f"gu{l}", w_gu[l], s_gu[l], i)
            wuu = unit_A(f"gu{l}", w_gu[l], s_gu[l], N_GU // 2 + i)
            for j in range(2):
                c = 2 * i + j
                psG = proj_fm(wg, j, hT)
                psU = proj_fm(wuu, j, hT)
                g = gsb[c % 2]
                a = acc[c % 2]
                s_ = sil[c % 2]
                cp('dve', g[:, 0:2], ghalo[:, c, :])
                act(g[:, 2:T + 2], psG[:], ACT.Copy)
                cp('act', ghalo[:, c, :], g[:, T:T + 2])
                ts('dve', a, g[:, 0:T], spk[:, fwb + c * 3:fwb + c * 3 + 1], spk[:, fbb + c:fbb + c + 1], ALU.mult, ALU.add)
                stt('dve', a, g[:, 1:T + 1], spk[:, fwb + c * 3 + 1:fwb + c * 3 + 2], a, ALU.mult, ALU.add)
                stt('dve', a, g[:, 2:T + 2], spk[:, fwb + c * 3 + 2:fwb + c * 3 + 3], a, ALU.mult, ALU.add)
                act(s_, a, ACT.Silu)
                tt('dve', Abuf[:, c, :], psU[:], s_, ALU.mult)
        sf = SSFuse() if fuse_next else None
        for m in range(KC):
            ps = psum.next()
            for hf in range(2):
                wd = unit_down(l, m, hf)
                for c2 in range(22):
                    mm(ps[:], wd[:, c2, :], Abuf[:, hf * 22 + c2, :], start=(hf == 0 and c2 == 0), stop=(hf == 1 and c2 == 21))
            tt('dve', xres[:, m, :], xres[:, m, :], ps[:], ALU.add)
            if sf:
                sf.chunk_done(m)
        if sf:
            sf.finish()
        ar.reset(m1)

    mA = ar.mark()
    stA = {}
    stA['R'] = ar.alloc([8, 128], F32)
    stA['Sbf'] = ar.alloc([8, 128], BF16)
    stA['xhalo'] = ar.alloc([8, 3], F32)
    stA['hst'] = ar.alloc([1, 8], F32)[:, 0, :]
    stA['ghalo'] = [ar.alloc([FC, 2], F32), None]
    stA['rot'] = ar.alloc([2, T], F32)
    mA2 = ar.mark()

    def layer0_tile(sq_i, ti):
        st = stA
        m1 = ar.mark()
        stg = [ar.alloc([1, D], F32)[:, 0, :] for _ in range(2)]
        for sub in range(NSUB):
            sg = stg[sub % 2]
            r0 = ti * T + sub * 128
            dma('pool', sg, x_in[sq_i, r0:r0 + 128, :], writes=[sg], chan=f"xl{sub % 2}")
            for c4 in range(4):
                ps = psum.next()
                for cc in range(4):
                    c = c4 * 4 + cc
                    tr(ps[:, cc * 128:(cc + 1) * 128], sg[:, c * 128:(c + 1) * 128], ident)
                cp('act' if c4 % 2 == 0 else 'dve', xres[:, c4 * 4:c4 * 4 + 4, sub * 128:(sub + 1) * 128],
                   ps[:].rearrange("p (a b) -> p a b", a=4))
        ar.reset(m1)
        dma('pool', st['rot'], rot_in[:, :, ti * T:(ti + 1) * T].rearrange("a p t -> p a t"), writes=[st['rot']], chan="rot")
        cosT = st['rot'][:, 0, :]
        sinS = st['rot'][:, 1, :]
        hT = ar.alloc([KC, T], BF16)
        mixed = ar.alloc([KC, T], BF16)
        scr = {'sq': [ar.alloc([1, T], BF16)[:, 0, :] for _ in range(2)], 'rstd': ar.alloc([1, T], F32)[:, 0, :]}
        if stop == 'load':
            ar.reset(m1)
            return
        rmsnorm(spf('nmix', 0, 16), hT, scr)
        if stop == 'norm':
            ar.reset(m1)
            return
        m2 = ar.mark()
        xr = ar.alloc([8, T + 3], F32)
        gy = ar.alloc([8, T], BF16)
        f32s = Rot([ar.alloc([1, T], F32)[:, 0, :] for _ in range(10)])
        xcbs = Rot([ar.alloc([1, T], BF16)[:, 0, :] for _ in range(2)])
        if ti == 0:
            memset('dve', st['xhalo'], 0.0)
            memset('dve', st['hst'], 0.0)
        for n in range(8):
            cp('dve', xr[:, n, 0:3], st['xhalo'][:, n, :])
        for u4 in range(4):
            wx_ = unit_A("ein", w_ein, s_ein, 16 + u4)
            wy_ = unit_A("ein", w_ein, s_ein, 20 + u4)
            for j in range(2):
                n = 2 * u4 + j
                psx = proj_fm(wx_, j, hT)
                act(xr[:, n, 3:T + 3], psx[:], ACT.Copy)
                psy = proj_fm(wy_, j, hT)
                act(gy[:, n, :], psy[:], ACT.Gelu_apprx_tanh)
                cp('act', st['xhalo'][:, n, :], xr[:, n, T:T + 3])
                cwb = SP_OFF['cw'] + n * 4
                xc = f32s.next()
                ts('dve', xc, xr[:, n, 0:T], spk[:, cwb:cwb + 1], spf('cb', n, n + 1), ALU.mult, ALU.add)
                for jj in range(1, 4):
                    stt('dve', xc, xr[:, n, jj:jj + T], spk[:, cwb + jj:cwb + jj + 1], xc, ALU.mult, ALU.add)
                xcb = xcbs.next()
                cp('act', xcb, xc)
                psa = psum.next()
                mm(psa[:], gatesb[:, 0, n, :], xcb)
                psi = psum.next()
                mm(psi[:], gatesb[:, 1, n, :], xcb)
                r_ = f32s.next()
                act(r_, psa[:], ACT.Sigmoid, bias=spf('ba', n, n + 1))
                i_ = f32s.next()
                act(i_, psi[:], ACT.Sigmoid, bias=spf('bx', n, n + 1))
                a_ = f32s.next()
                act(a_, r_, ACT.Exp, scale=clam[:, n:n + 1])
                a2 = f32s.next()
                act(a2, r_, ACT.Exp, scale=clam[:, 8 + n:9 + n])
                ts('dve', a2, a2, -1.0, 1.0, ALU.mult, ALU.add)
                ts('dve', a2, a2, 0.0, None, ALU.max)
                act(a2, a2, ACT.Sqrt)
                tt('dve', i_, i_, xc, ALU.mult)
                tt('dve', i_, i_, a2, ALU.mult)
                P.add('dve', (lambda a_, i_, n: lambda e: e.tensor_tensor_scan(out=a_, data0=a_, data1=i_, initial=st['hst'][:, n:n + 1],
                                                                               op0=ALU.mult, op1=ALU.add))(a_, i_, n),
                      reads=[a_, i_, st['hst'][:, n:n + 1]], writes=[a_])
                cp('act', st['hst'][:, n:n + 1], a_[:, T - 1:T])
                tt('dve', mixed[:, 8 + n, :], a_, gy[:, n, :], ALU.mult)
        ar.reset(m2)
        if stop == 'lru':
            ar.reset(m1)
            return
        qd = ar.alloc([4, T], BF16)
        kd = ar.alloc([4, T], BF16)
        kdt = ar.alloc([NSUB, 512], BF16)
        vt = ar.alloc([NSUB, 512], BF16)
        Gs = ar.alloc([4, T], BF16)
        qsbs = Rot([ar.alloc([1, T], BF16)[:, 0, :] for _ in range(2)])
        f32r = Rot([ar.alloc([1, T], F32)[:, 0, :] for _ in range(6)])
        STs = Rot([ar.alloc([1, T], BF16)[:, 0, :] for _ in range(2)])
        if ti == 0 and not os.environ.get('K_NOMEMSET'):
            memset('dve', st['R'], 0.0)
            memset('dve', st['Sbf'], 0.0)

        def rotary(ps, dst, dec):
            qs = qsbs.next()
            t1 = f32r.next()
            t2 = f32r.next()
            tt('dve', t1, ps[:], cosT, ALU.mult)
            P.add('act', lambda e: e.activation(out=qs, in_=ps[:], func=ACT.Copy), reads=[ps[:], t1], writes=[qs])
            if not os.environ.get('K_NOPERM'):
                ps2 = psum.next()
                mm(ps2[:], permb[:], qs)
                tt('dve', t2, ps2[:], sinS, ALU.mult)
                tt('dve', t1, t1, t2, ALU.add)
            if os.environ.get('K_NOBC'):
                cp('dve', dst, t1)
                return
            tt('dve', dst.rearrange("p (a b) -> p a b", a=4), t1.rearrange("p (a b) -> p a b", a=4),
               dec.unsqueeze(1).to_broadcast([128, 4, 128]), ALU.mult)

        for hb in range(2):
            for uu in range(2):
                wq = unit_A("ein", w_ein, s_ein, hb * 2 + uu)
                for j in range(2):
                    hl = uu * 2 + j
                    h = hb * 4 + hl
                    ps = proj_fm(wq, j, hT)
                    rotary(ps, qd[:, hl, :], csf('qdec', h * 128, (h + 1) * 128))
            if stop == 'ret1':
                ar.reset(m1)
                return
            for uu in range(2):
                wk = unit_A("ein", w_ein, s_ein, 4 + hb * 2 + uu)
                for j in range(2):
                    hl = uu * 2 + j
                    h = hb * 4 + hl
                    ps = proj_fm(wk, j, hT)
                    rotary(ps, kd[:, hl, :], csf('kdec', h * 128, (h + 1) * 128))
                    pst = psum.next()
                    for sub in range(NSUB):
                        mm(pst[:, sub * 128:(sub + 1) * 128], kd[:, hl, sub * 128:(sub + 1) * 128], identb[:])
                    cp('act', kdt[:, :, hl * 128:(hl + 1) * 128], pst[:].rearrange("p (a b) -> p a b", a=4))
            if stop == 'ret2':
                ar.reset(m1)
                return
            for uu in range(2):
                wv = unit_A("ein", w_ein, s_ein, 8 + hb * 2 + uu)
                for sub in range(NSUB):
                    ps = psum.next()
                    for k in range(KC):
                        mm(ps[:, 0:256], hT[:, k, sub * 128:(sub + 1) * 128], wv[:, k, :], start=(k == 0), stop=(k == KC - 1))
                    cp('act' if sub % 2 == 0 else 'dve', vt[:, sub, uu * 256:(uu + 1) * 256], ps[:, 0:256])
            if stop == 'ret3':
                ar.reset(m1)
                return
            for uu in range(2):
                wg_ = unit_A("ein", w_ein, s_ein, 12 + hb * 2 + uu)
                for j in range(2):
                    hl = uu * 2 + j
                    h = hb * 4 + hl
                    ps = proj_fm(wg_, j, hT)
                    sl = f32r.next()
                    act(sl, ps[:], ACT.Silu)
                    ts('dve', Gs[:, hl, :], sl, spf('gn', h, h + 1), None, ALU.mult)
            if stop == 'ret4':
                ar.reset(m1)
                return
            for n in range(NSUB):
                cols = slice(n * 128, (n + 1) * 128)
                psS = psum.next()
                for hl in range(4):
                    mm(psS[:, hl * 128:(hl + 1) * 128], kd[:, hl, cols], qd[:, hl, cols])
                psKV = psum.next()
                for hl in range(4):
                    mm(psKV[:, hl * 128:(hl + 1) * 128], kdt[:, n, hl * 128:(hl + 1) * 128], vt[:, n, hl * 128:(hl + 1) * 128])
                STb = STs.next()
                tt('dve', STb.rearrange("p (a b) -> p a b", a=4), psS[:].rearrange("p (a b) -> p a b", a=4),
                   csf('triu').unsqueeze(1).to_broadcast([128, 4, 128]), ALU.mult)
                psY = psum.next()
                for hl in range(4):
                    h = hb * 4 + hl
                    mm(psY[:, hl * 128:(hl + 1) * 128], vt[:, n, hl * 128:(hl + 1) * 128], STb[:, hl * 128:(hl + 1) * 128], start=True, stop=False)
                    mm(psY[:, hl * 128:(hl + 1) * 128], st['Sbf'][:, h, :], qd[:, hl, cols], start=False, stop=True)
                for hl in range(4):
                    h = hb * 4 + hl
                    stt('dve', st['R'][:, h, :], st['R'][:, h, :], float(CDEC[h]), psKV[:, hl * 128:(hl + 1) * 128], ALU.mult, ALU.add)
                    act(st['Sbf'][:, h, :], st['R'][:, h, :], ACT.Copy, scale=float(CDEC[h]))
                ysq = qsbs.next()
                act(ysq, psY[:], ACT.Square)
                psN = psum.next()
                mm(psN[:], onesb[:], ysq)
                rs = f32r.next()
                ts('dve', rs, psN[:], 1.0 / 128, EPS, ALU.mult, ALU.add)
                act(rs, rs, ACT.Sqrt)
                recip(rs, rs)
                yn = f32r.next()
                tt('dve', yn, psY[:], rs, ALU.mult)
                tt('dve', mixed[:, hb * 4:hb * 4 + 4, cols], yn.rearrange("p (a b) -> p a b", a=4), Gs[:, :, cols], ALU.mult)
        if stop == 'ret':
            ar.reset(m1)
            return
        residual_proj("eout", w_eout, s_eout, N_EOUT, mixed)
        ar.reset(m1)

    def alloc_passB():
        st = {}
        st['Kc'] = ar.alloc([4, S], BF16)
        st['Vc'] = ar.alloc([S // 128, 512], BF16)
        st['ki2'] = ar.alloc([1, S], BF16)[:, 0, :]
        st['ghalo'] = [None, ar.alloc([FC, 2], F32)]
        st['bt'] = [ar.alloc([1, 256], F32)[:, 0, :] for _ in range(2)]
        return st

    def layer1_tile(st, sq_i, ti):
        m1 = ar.mark()
        selT = ar.alloc([S // 128, T], BF16)
        qF = ar.alloc([16, T], BF16)
        m_h = ar.mark()
        hT_lo = ar.mark()
        hT = ar.alloc([KC, T], BF16)
        attn = hT
        qiF = ar.alloc([8, T], BF16)
        wi = ar.alloc([NSUB, 16], F32)
        scr = {'sq': [ar.alloc([1, T], BF16)[:, 0, :] for _ in range(2)], 'rstd': ar.alloc([1, T], F32)[:, 0, :]}
        rmsnorm(spf('nmix', 16, 32), hT, scr)
        tcols = slice(ti * T, (ti + 1) * T)
        for u in range(8):
            wq = unit_A("oin", w_oin, s_oin, u)
            for j in range(2):
                h = 2 * u + j
                ps = proj_fm(wq, j, hT)
                act(qF[:, h, :], ps[:], ACT.Copy, scale=128.0 ** -0.5)
        for u in range(2):
            wk = unit_A("oin", w_oin, s_oin, 8 + u)
            for j in range(2):
                kvh = 2 * u + j
                ps = proj_fm(wk, j, hT)
                cp('dve', st['Kc'][:, kvh, tcols], ps[:])
        for u in range(2):
            wv = unit_A("oin", w_oin, s_oin, 10 + u)
            for sub in range(NSUB):
                ps = psum.next()
                for k in range(KC):
                    mm(ps[:, 0:256], hT[:, k, sub * 128:(sub + 1) * 128], wv[:, k, :], start=(k == 0), stop=(k == KC - 1))
                cp('act' if sub % 2 == 0 else 'dve', st['Vc'][:, ti * NSUB + sub, u * 256:(u + 1) * 256], ps[:, 0:256])
        for u in range(4):
            wqi = unit_A("oin", w_oin, s_oin, 12 + u)
            for j in range(2):
                cq = 2 * u + j
                ps = proj_fm(wqi, j, hT)
                cp('act', qiF[:, cq, :], ps[:])
        wl = unit_oin_last()
        ps = proj_fm(wl, 0, hT)
        cp('dve', st['ki2'][:, tcols], ps[:])
        for sub in range(NSUB):
            ps = psum.next()
            for k in range(KC):
                mm(ps[:, 0:16], hT[:, k, sub * 128:(sub + 1) * 128], wl[:, k, 128:144], start=(k == 0), stop=(k == KC - 1))
            act(wi[:, sub, :], ps[:, 0:16], ACT.Copy, scale=1.0 / 32.0)
        m2 = ar.mark()
        Lt = T * (ti + 1)
        na = Lt // 128
        scoreA = ar.alloc([1, S], F32)[:, 0, :]
        scoreB = arena[:, hT_lo // 2:(hT_lo + 4 * S) // 2].bitcast(F32)
        selb = [ar.alloc([1, S], BF16)[:, 0, :] for _ in range(2)]
        junk = selb[1]
        Rb = Rot([ar.alloc([1, T], F32)[:, 0, :] for _ in range(3)])
        smb = [ar.alloc([1, 8 + NIT], F32)[:, 0, :] for _ in range(2)]
        todo = []
        for sub in range(NSUB):
            gt = ti * NSUB + sub
            scol = slice(sub * 128, (sub + 1) * 128)
            if gt < 2:
                for a in range(na):
                    dst = selT[:, a, scol]
                    if a < gt:
                        cp('act', dst, onesb[:])
                    elif a == gt:
                        cp('act', dst, trib[:])
                    else:
                        memset('dve', dst, 0.0)
            else:
                todo.append(sub)
        for p0 in range(0, len(todo), 2):
            pair = todo[p0:p0 + 2]
            scs = [scoreA, scoreB]
            Ls = []
            for ix, sub in enumerate(pair):
                gt = ti * NSUB + sub
                scol = slice(sub * 128, (sub + 1) * 128)
                score = scs[ix]
                sm = smb[ix]
                L = 128 * (gt + 1)
                Ls.append(L)
                nblk = (L + 511) // 512
                for sbk in range(nblk):
                    w = min(512, L - 512 * sbk)
                    blk = slice(sbk * 512, sbk * 512 + w)
                    for h in range(16):
                        cq = h // 2
                        rows = slice((h % 2) * 64, (h % 2) * 64 + 64)
                        ps = psum.next()
                        mm(ps[:, 0:w], qiF[rows, cq, scol], st['ki2'][rows, blk])
                        rb = Rb.next()
                        act(rb[:, 0:w], ps[:, 0:w], ACT.Relu)
                        if h == 0:
                            ts('dve', score[:, blk], rb[:, 0:w], wi[:, sub, 0:1], None, ALU.mult)
                        else:
                            stt('dve', score[:, blk], rb[:, 0:w], wi[:, sub, h:h + 1], score[:, blk], ALU.mult, ALU.add)
                ts('dve', junk[:, 0:L], score[:, 0:L], 1.0, None, ALU.mult, ALU.max, accum=sm[:, 0:1])
                ts('dve', junk[:, 0:L], score[:, 0:L], 1.0, None, ALU.mult, ALU.min, accum=sm[:, 1:2])
                tt('dve', score[:, L - 128:L], score[:, L - 128:L], csf('negm'), ALU.add)
                if L < Lt:
                    memset('dve', score[:, L:Lt], NEG)
                tt('dve', sm[:, 5:6], sm[:, 0:1], sm[:, 1:2], ALU.subtract)
                ts('dve', sm[:, 8:8 + NIT], csf('pow2'), sm[:, 5:6], None, ALU.mult)
            for k in range(NIT):
                for ix in range(len(pair)):
                    sm = smb[ix]
                    tt('dve', sm[:, 2:3], sm[:, 1:2], sm[:, 8 + k:9 + k], ALU.add)
                for ix in range(len(pair)):
                    sm = smb[ix]
                    ts('dve', junk[:, 0:Ls[ix]], scs[ix][:, 0:Ls[ix]], sm[:, 2:3], None, ALU.is_ge, ALU.add, accum=sm[:, 3:4])
                for ix in range(len(pair)):
                    sm = smb[ix]
                    stt('dve', sm[:, 4:5], sm[:, 3:4], 256.0, sm[:, 8 + k:9 + k], ALU.is_ge, ALU.mult)
                for ix in range(len(pair)):
                    sm = smb[ix]
                    tt('dve', sm[:, 1:2], sm[:, 1:2], sm[:, 4:5], ALU.add)
            for ix, sub in enumerate(pair):
                scol = slice(sub * 128, (sub + 1) * 128)
                sel = selb[ix]
                ts('dve', sel[:, 0:Lt], scs[ix][:, 0:Lt], smb[ix][:, 1:2], None, ALU.is_ge)
                for a0 in range(0, na, 4):
                    n4 = min(4, na - a0)
                    pst = psum.next()
                    for a in range(n4):
                        mm(pst[:, a * 128:(a + 1) * 128], sel[:, (a0 + a) * 128:(a0 + a + 1) * 128], identb[:])
                    cp('act', selT[:, a0:a0 + n4, scol], pst[:, 0:n4 * 128].rearrange("p (a b) -> p a b", a=n4))
        ar.reset(m2)
        Eb = Rot([ar.alloc([1, T], BF16)[:, 0, :] for _ in range(3)])
        Pm = Rot([ar.alloc([1, T], BF16)[:, 0, :] for _ in range(4)])
        tf = Rot([ar.alloc([1, 256], F32)[:, 0, :] for _ in range(2)])
        rz = Rot([ar.alloc([1, T], F32)[:, 0, :] for _ in range(2)])
        acc_banks = Rot(psb[0:4])
        tmp_banks = Rot(psb[4:8])

        def flush_one(pend, psO, psZ, kvh):
            a_, c0_, pm_ = pend.pop(0)
            mm(psO[:, c0_:T], st['Vc'][:, a_, kvh * 128:(kvh + 1) * 128], pm_[:, c0_:T], start=(a_ == 0), stop=(a_ == na - 1))
            mm(psZ[:, c0_:T], onesb[:], pm_[:, c0_:T], start=(a_ == 0), stop=(a_ == na - 1))
        for hq in range(16):
            kvh = hq // 4
            btb = st['bt'][hq % 2]
            dma('pool', btb, bt_in[hq], writes=[btb], chan=f"bt{hq % 2}")
            psO = acc_banks.next()
            psZ = acc_banks.next()
            pend = []
            for a in range(na):
                sa = a - ti * NSUB
                c0 = max(0, sa) * 128
                psL = tmp_banks.next()
                mm(psL[:, c0:T], st['Kc'][:, kvh, a * 128:(a + 1) * 128], qF[:, hq, c0:T])
                eb = Eb.next()
                n1 = c0
                if sa >= -1:
                    n0 = max(0, sa) * 128
                    n1 = min(NSUB, sa + 2) * 128
                    tfb = tf.next()
                    tt('dve', tfb[:, 0:n1 - n0], psL[:, n0:n1], btb[:, n0 - 128 * sa:n1 - 128 * sa], ALU.add)
                    act(eb[:, n0:n1], tfb[:, 0:n1 - n0], ACT.Exp)
                if n1 < T:
                    act(eb[:, n1:T], psL[:, n1:T], ACT.Exp, bias=csf('c31', hq, hq + 1))
                pm = Pm.next()
                tt('dve', pm[:, c0:T], eb[:, c0:T], selT[:, a, c0:T], ALU.mult)
                pend.append((a, c0, pm))
                if len(pend) > 2:
                    flush_one(pend, psO, psZ, kvh)
            while pend:
                flush_one(pend, psO, psZ, kvh)
            r = rz.next()
            recip(r, psZ[:])
            tt('dve', attn[:, hq, :], psO[:], r, ALU.mult)
        residual_proj("oout", w_oout, s_oout, N_OOUT, attn)
        ar.reset(m1)

    def final_tile(sq_i, ti):
        m1 = ar.mark()
        scr = {'sq': [ar.alloc([1, T], BF16)[:, 0, :] for _ in range(2)], 'rstd': ar.alloc([1, T], F32)[:, 0, :]}
        ostg = [ar.alloc([1, D], F32)[:, 0, :] for _ in range(2)]
        ss = ssb
        rs = scr['rstd']
        ts('dve', rs, ss[:], 1.0 / D, EPS, ALU.mult, ALU.add)
        act(rs, rs, ACT.Sqrt)
        recip(rs, rs)
        for c in range(KC):
            stt('dve', xres[:, c, :], xres[:, c, :], spf('fnorm', c, c + 1), rs, ALU.mult, ALU.mult)
        chs = []
        for sub in range(NSUB):
            og = ostg[sub % 2]
            for c4 in range(4):
                ps = psum.next()
                for cc in range(4):
                    c = c4 * 4 + cc
                    tr(ps[:, cc * 128:(cc + 1) * 128], xres[:, c, sub * 128:(sub + 1) * 128], ident)
                cp('act' if c4 % 2 == 0 else 'dve', og[:, c4 * 512:(c4 + 1) * 512], ps[:])
            r0 = ti * T + sub * 128
            chs.append(dma('pool', out[sq_i, r0:r0 + 128, :], og, reads=[og], chan=f"os{sub % 2}"))
        ar.reset(m1)
        return chs

    final_chans = set()
    for sq_i in range(nseq):
        ar.reset(mA2)
        for ti in range(NTL):
            if ti == 0:
                memset('dve', stA['ghalo'][0], 0.0)
            layer0_tile(sq_i, ti)
            if stop is None or stop == 'ffn':
                ffn(0, stA)
            key = f"x2:{sq_i}:{ti}"
            ch = dma('pool', x2s[sq_i, ti], xres[:].rearrange("p a b -> p (a b)"), reads=[xres[:]], writes=[key], chan="x2st")
            if debug_stage == 'A':
                final_chans.add(ch)
        if debug_stage == 'A':
            continue
        ar.reset(mA)
        stB = alloc_passB()
        for ti in range(NT):
            key = f"x2:{sq_i}:{ti}"
            dma('pool', xres[:].rearrange("p a b -> p (a b)"), x2s[sq_i, ti], reads=[key], writes=[xres[:]], chan="x2ld")
            if ti == 0:
                memset('dve', stB['ghalo'][1], 0.0)
            layer1_tile(stB, sq_i, ti)
            ffn(1, stB, fuse_next=True)
            for ch in final_tile(sq_i, ti):
                final_chans.add(ch)
        ar.reset(mA)
        ar.reset(mA2)
    P.emit(final_chans=sorted(final_chans))
    return nc, P


def _fm(v, nchunk):
    return np.ascontiguousarray(np.asarray(v, np.float32).reshape(nchunk, 128).T)


def _t5_bucket(n):
    n = np.maximum(n, 0)
    nf = np.maximum(n, 16).astype(np.float32)
    large = 16 + (np.log(nf / np.float32(16)) / np.float32(math.log(128 / 16)) * np.float32(16)).astype(np.int32)
    large = np.minimum(large, 31)
    return np.where(n < 16, n, large)


def host_prep(inp):
    f = np.float32
    sp = np.zeros((128, SP_W), f)

    def put(name, arr):
        arr = np.asarray(arr, f).reshape(128, -1)
        sp[:, SP_OFF[name]:SP_OFF[name] + arr.shape[1]] = arr

    put('nmix', np.stack([_fm(inp['norm_mix'][l], 16) for l in range(2)], 1))
    put('nffn', np.stack([_fm(inp['norm_ffn'][l], 16) for l in range(2)], 1))
    put('fnorm', _fm(inp['final_norm'], 16))
    put('gn', _fm(inp['e_ret_gn'][0], 8))
    put('cw', np.asarray(inp['e_conv_w'][0], f).reshape(4, 8, 128).transpose(2, 1, 0))
    put('cb', _fm(inp['e_conv_b'][0], 8))
    put('ba', _fm(inp['e_gate_a_b'][0], 8))
    put('bx', _fm(inp['e_gate_x_b'][0], 8))
    put('lam', _fm(inp['e_lambda'][0], 8))
    put('fw', np.asarray(inp['ffn_conv_w'], f).reshape(2, 3, FC, 128).transpose(3, 0, 2, 1))
    put('fb', np.asarray(inp['ffn_conv_b'], f).reshape(2, FC, 128).transpose(2, 0, 1))

    cs = np.zeros((128, CS_W), f)

    def putc(name, arr):
        arr = np.asarray(arr, f).reshape(128, -1)
        cs[:, CS_OFF[name]:CS_OFF[name] + arr.shape[1]] = arr

    idx = np.arange(128)
    putc('ident', np.eye(128, dtype=f))
    putc('triu', (idx[None, :] >= idx[:, None]).astype(f))
    perm = np.zeros((128, 128), f)
    perm[(idx + 64) % 128, idx] = 1.0
    putc('perm', perm)
    putc('negm', np.where(idx[None, :] > idx[:, None], f(NEG), f(0)))
    g64 = np.array(GAMMA, np.float64)
    i64 = np.arange(128, dtype=np.float64)
    qdec = g64[:, None] ** (i64[None, :] + 1.0)
    kdec = g64[:, None] ** (-(i64[None, :] + 1.0)) * (128.0 ** -0.5)
    putc('qdec', np.broadcast_to(qdec.reshape(1, 1024), (128, 1024)))
    putc('kdec', np.broadcast_to(kdec.reshape(1, 1024), (128, 1024)))
    putc('c31', np.broadcast_to(np.asarray(inp['rel_bias'], f)[31][None, :], (128, 16)))
    putc('pow2', np.broadcast_to((2.0 ** (-(np.arange(NIT, dtype=np.float64) + 1.0)))[None, :], (128, NIT)))

    gates = np.stack([np.asarray(inp['e_gate_a_w'][0], f).transpose(1, 0, 2),
                      np.asarray(inp['e_gate_x_w'][0], f).transpose(1, 0, 2)], 1)
    half = 64
    freqs = (f(10000.0) ** (-np.arange(half, dtype=f) / f(half))).astype(f)
    ang = (np.arange(S, dtype=f)[:, None] * freqs[None, :]).astype(f)
    cos = np.cos(ang).astype(f).T
    sin = np.sin(ang).astype(f).T
    rot = np.stack([np.concatenate([cos, cos], 0), np.concatenate([-sin, sin], 0)], 0)
    rel = idx[None, :256 - 128 + 128] if False else (np.arange(256)[None, :] - idx[:, None])
    bidx = _t5_bucket(rel)
    bt = np.ascontiguousarray(np.asarray(inp['rel_bias'], f)[bidx].transpose(2, 0, 1))
    return dict(smallp=sp, consts=cs, gates=np.ascontiguousarray(gates), rot=np.ascontiguousarray(rot), bt=bt)


_CACHE = {}


def kernel(**inputs):
    n_cores = 8
    inp = {k: np.asarray(v) for k, v in inputs.items()}
    hp = host_prep(inp)
    shared = dict(hp)
    shared['e_w_in'] = np.ascontiguousarray(inp['e_w_in'][0], dtype=np.float32)
    shared['e_w_out'] = np.ascontiguousarray(inp['e_w_out'][0], dtype=np.float32)
    shared['o_w_in'] = np.ascontiguousarray(inp['o_w_in'][0], dtype=np.float32)
    shared['o_w_out'] = np.ascontiguousarray(inp['o_w_out'][0], dtype=np.float32)
    shared['ffn_w_gu'] = np.ascontiguousarray(inp['ffn_w_gu'], dtype=np.float32)
    shared['ffn_w_down'] = np.ascontiguousarray(inp['ffn_w_down'], dtype=np.float32)
    x = np.ascontiguousarray(inp['x'], dtype=np.float32)
    if 'nc' not in _CACHE:
        _CACHE['nc'] = build_program()[0]
    nc = _CACHE['nc']
    in_maps = []
    for c in range(n_cores):
        m = dict(shared)
        m['x'] = x[c * NSEQ:(c + 1) * NSEQ]
        in_maps.append(m)
    res = run_bass_kernel_spmd(nc, in_maps, core_ids=list(range(n_cores)))
    return np.concatenate([r['out'] for r in res.results], axis=0)
```

```python
import math
import os
import numpy as np
import concourse.bass as bass
import concourse.mybir as mybir
from concourse.bass_utils import run_bass_kernel_spmd

ACT = mybir.ActivationFunctionType
ALU = mybir.AluOpType
F32 = mybir.dt.float32
BF16 = mybir.dt.bfloat16

CELL = 256
STRICT = bool(os.environ.get('K_STRICT'))
RAW_WINDOW = 2
EPOCH = 30000
ENGS = ('pe', 'act', 'dve', 'pool', 'sp')


class Op:
    __slots__ = ('eng', 'fn', 'deps', 'signal', 'sig', 'is_dma', 'chan', 'dval', 'idx')

    def __init__(self, eng, fn, is_dma=False, chan=None):
        self.eng = eng
        self.fn = fn
        self.deps = []
        self.signal = False
        self.sig = 0
        self.is_dma = is_dma
        self.chan = chan
        self.dval = 0
        self.idx = 0


class Prog:
    def __init__(self, nc):
        self.nc = nc
        self.ops = {e: [] for e in ENGS}
        self.cells = {}
        self.chan_last = {}
        self.chan_cnt = {}
        self.n_ops = 0

    def _cells(self, ap):
        if isinstance(ap, str):
            return [('k', ap)]
        t = ap.tensor
        if 'DRam' in type(t).__name__:
            return []
        esz = mybir.dt.size(ap.dtype)
        pat = ap.ap
        pstep = pat[0][0]
        off = ap.offset
        if pstep > 0:
            off = off % pstep
        lo = off
        hi = off
        for (st, cnt) in pat[1:]:
            if st >= 0:
                hi += st * (cnt - 1)
            else:
                lo += st * (cnt - 1)
        lo_b = lo * esz
        hi_b = (hi + 1) * esz
        name = t.name
        if name.startswith('psb'):
            return [(name, 0)]
        return [(name, c) for c in range(lo_b // CELL, (hi_b - 1) // CELL + 1)]

    def add(self, eng, fn, reads=(), writes=(), chan=None):
        is_dma = chan is not None
        op = Op(eng, fn, is_dma, chan)
        self.n_ops += 1
        deps = {}

        def dep(d):
            if d is None or d is op:
                return
            if d.is_dma:
                deps[('dma', id(d))] = d
            else:
                k = ('c', d.eng)
                o = deps.get(k)
                if o is None or d.idx > o.idx:
                    deps[k] = d

        rcells = []
        for r in reads:
            if r is not None and not isinstance(r, (int, float)):
                rcells.extend(self._cells(r))
        wcells = []
        for w in writes:
            wcells.extend(self._cells(w))
        cells = self.cells
        nxt = len(self.ops[eng])
        for c in rcells:
            rec = cells.get(c)
            if rec is not None:
                w = rec[0]
                if w is not None and (STRICT or w.is_dma or is_dma or w.eng != eng or nxt - w.idx <= RAW_WINDOW):
                    dep(w)
                if c[0].startswith('psb'):
                    for e2, r in rec[1].items():
                        if e2 != eng:
                            dep(r)
        for c in wcells:
            rec = cells.get(c)
            if rec is not None:
                w = rec[0]
                if w is not None and (w.is_dma or is_dma or w.eng != eng or (STRICT and eng != 'pe')):
                    dep(w)
                for e2, r in rec[1].items():
                    if e2 != eng or is_dma or (STRICT and eng != 'pe'):
                        dep(r)
                for r in rec[2]:
                    dep(r)
        if is_dma:
            prev = self.chan_last.get(chan)
            if prev is not None:
                dep(prev)
            self.chan_last[chan] = op
            n = self.chan_cnt.get(chan, 0) + 1
            self.chan_cnt[chan] = n
            op.dval = 16 * n
        lst = self.ops[eng]
        op.idx = len(lst)
        lst.append(op)
        for d in deps.values():
            if not d.is_dma:
                d.signal = True
            op.deps.append(d)
        for c in wcells:
            cells[c] = [op, {}, []]
        for c in rcells:
            rec = cells.get(c)
            if rec is None:
                rec = [None, {}, []]
                cells[c] = rec
            if is_dma:
                rec[2].append(op)
            else:
                rec[1][eng] = op
        return op

    def emit(self, final_chans=()):
        nc = self.nc
        from contextlib import ExitStack
        with ExitStack() as es:
            nsig = {}
            for e in ENGS:
                n = 0
                for op in self.ops[e]:
                    if op.signal and not op.is_dma:
                        n += 1
                        op.sig = n
                nsig[e] = n
            sems = {}
            for e in ENGS:
                ne = max((nsig[e] + EPOCH - 1) // EPOCH, 1)
                sems[e] = [es.enter_context(nc.semaphore(f'c_{e}_{i}')) for i in range(ne)]
            chans = {}
            for ch in self.chan_cnt:
                chans[ch] = es.enter_context(nc.semaphore(f'd_{ch}'))
            block = es.enter_context(nc.Block())

            def make(e):
                def body(eng):
                    waited_c = {}
                    waited_d = {}
                    for op in self.ops[e]:
                        for d in op.deps:
                            if d.is_dma:
                                if waited_d.get(d.chan, 0) < d.dval:
                                    eng.wait_ge(chans[d.chan], d.dval)
                                    waited_d[d.chan] = d.dval
                            else:
                                if waited_c.get(d.eng, 0) < d.sig:
                                    ep = (d.sig - 1) // EPOCH
                                    eng.wait_ge(sems[d.eng][ep], (d.sig - 1) % EPOCH + 1)
                                    waited_c[d.eng] = d.sig
                        ins = op.fn(eng)
                        if op.is_dma:
                            ins.then_inc(chans[op.chan], 16)
                        elif op.signal:
                            ep = (op.sig - 1) // EPOCH
                            ins.then_inc(sems[e][ep], 1)
                    if e == 'sp':
                        for ch in final_chans:
                            eng.wait_ge(chans[ch], 16 * self.chan_cnt[ch])
                return body

            block.tensor(make('pe'))
            block.scalar(make('act'))
            block.vector(make('dve'))
            block.gpsimd(make('pool'))
            block.sync(make('sp'))


D = 2048
S = 2048
NSEQ = 2
T = 512
NT = S // T
NSUB = T // 128
KC = D // 128
E_IN = 6144
O_IN = 4176
DFF = 5632
FC = DFF // 128
EPS = 1e-6
NSLOT = 4
SLOT = 4096
NIT = 16
NEG = -1e30

SP_FIELDS = [('nmix', 32), ('nffn', 32), ('fnorm', 16), ('gn', 8), ('cw', 32), ('cb', 8), ('ba', 8),
             ('bx', 8), ('lam', 8), ('fw', 2 * FC * 3), ('fb', 2 * FC)]
SP_OFF = {}
_o = 0
for _n, _w in SP_FIELDS:
    SP_OFF[_n] = _o
    _o += _w
SP_W = _o
CS_FIELDS = [('ident', 128), ('triu', 128), ('perm', 128), ('negm', 128), ('qdec', 1024), ('kdec', 1024),
             ('c31', 16), ('pow2', NIT)]
CS_OFF = {}
_o = 0
for _n, _w in CS_FIELDS:
    CS_OFF[_n] = _o
    _o += _w
CS_W = _o

GAMMA = [1.0 - 2.0 ** (-5.0 - h) for h in range(8)]
CDEC = [g ** 128 for g in GAMMA]

N_EIN = E_IN // 256
N_EOUT = 8
N_GU = (2 * DFF) // 256
N_DOWN = 32
N_OIN = 17
N_OOUT = 8


def build_program(nseq=NSEQ, debug_stage=None, stop=None, ntiles=None):
    NTL = NT if ntiles is None else ntiles
    nc = bass.Bass("TRN2", target_bir_lowering=False)
    P = Prog(nc)

    def din(name, shape, dt=F32):
        return nc.dram_tensor(name, list(shape), dt, kind="ExternalInput").ap()

    x_in = din("x", [nseq, S, D])
    w_ein = din("e_w_in", [D, E_IN])
    w_eout = din("e_w_out", [D, D])
    w_oin = din("o_w_in", [D, O_IN])
    w_oout = din("o_w_out", [D, D])
    w_gu = din("ffn_w_gu", [2, D, 2 * DFF])
    w_dn = din("ffn_w_down", [2, DFF, D])
    sp_in = din("smallp", [128, SP_W])
    cs_in = din("consts", [128, CS_W])
    gates_in = din("gates", [128, 2, 8, 128])
    rot_in = din("rot", [2, 128, S])
    bt_in = din("bt", [16, 128, 256])
    out = nc.dram_tensor("out", [nseq, S, D], F32, kind="ExternalOutput").ap()

    def dscr(name, shape, dt):
        return nc.dram_tensor(name, list(shape), dt, kind="Internal").ap()

    s_ein = dscr("s_ein", [N_EIN, 128, SLOT], BF16)
    s_eout = dscr("s_eout", [N_EOUT, 128, SLOT], BF16)
    s_oin = dscr("s_oin", [N_OIN, 128, SLOT], BF16)
    s_oout = dscr("s_oout", [N_OOUT, 128, SLOT], BF16)
    s_gu = dscr("s_gu", [2, N_GU, 128, SLOT], BF16)
    s_dn = dscr("s_dn", [2, N_DOWN, 128, 22 * 128], BF16)
    if debug_stage == 'A':
        x2s = nc.dram_tensor("x2s", [nseq, NT, 128, KC * T], F32, kind="ExternalOutput").ap()
    else:
        x2s = dscr("x2s", [nseq, NT, 128, KC * T], F32)

    sb = nc.alloc_sbuf_tensor
    ring = sb("ring", [128, NSLOT, SLOT], BF16)
    xres = sb("xres", [128, KC, T], F32)
    spk = sb("spk", [128, SP_W], F32)
    cst = sb("cst", [128, CS_W], F32)
    identb = sb("identb", [128, 128], BF16)
    permb = sb("permb", [128, 128], BF16)
    onesb = sb("onesb", [128, 128], BF16)
    trib = sb("trib", [128, 128], BF16)
    gatesb = sb("gatesb", [128, 2, 8, 128], BF16)
    clam = sb("clam", [128, 16], F32)
    ARENA = 122 * 1024
    arena = sb("arena", [128, ARENA // 2], BF16)
    psb = [nc.alloc_psum_tensor(f"psb{i}", [128, 512], F32) for i in range(8)]

    def spf(name, lo=0, hi=None):
        o = SP_OFF[name]
        w = dict(SP_FIELDS)[name]
        hi = w if hi is None else hi
        return spk[:, o + lo:o + hi]

    def csf(name, lo=0, hi=None):
        o = CS_OFF[name]
        w = dict(CS_FIELDS)[name]
        hi = w if hi is None else hi
        return cst[:, o + lo:o + hi]

    ident = csf('ident')

    class Arena:
        def __init__(self):
            self.top = 0

        def alloc(self, shape, dt):
            n = 1
            for s_ in shape:
                n *= s_
            esz = mybir.dt.size(dt)
            nbytes = ((n * esz + 255) // 256) * 256
            lo = self.top
            self.top += nbytes
            assert self.top <= ARENA, f"arena overflow {self.top}"
            v = arena[:, lo // 2:(lo + n * esz) // 2]
            if dt != BF16:
                v = v.bitcast(dt)
            if len(shape) == 2:
                return v.rearrange("p (a b) -> p a b", a=shape[0])
            if len(shape) == 3:
                return v.rearrange("p (a b c) -> p a b c", a=shape[0], b=shape[1])
            return v

        def mark(self):
            return self.top

        def reset(self, m):
            self.top = m

    ar = Arena()

    class Rot:
        def __init__(self, items):
            self.items = items
            self.i = 0

        def next(self):
            v = self.items[self.i % len(self.items)]
            self.i += 1
            return v

    psum = Rot(psb[0:7])
    ssb = psb[7]

    def mm(o, lhsT, rhs, start=True, stop=True):
        P.add('pe', lambda e: e.matmul(o, lhsT=lhsT, rhs=rhs, start=start, stop=stop), reads=[lhsT, rhs], writes=[o])

    def tr(o, in_, idn):
        P.add('pe', lambda e: e.transpose(o, in_, idn), reads=[in_, idn], writes=[o])

    def act(o, in_, func, bias=None, scale=None):
        kw = {}
        if bias is not None:
            kw['bias'] = bias
        if scale is not None:
            kw['scale'] = scale
        P.add('act', lambda e: e.activation(out=o, in_=in_, func=func, **kw), reads=[in_, bias, scale], writes=[o])

    def tt(eng, o, a, b, op):
        P.add(eng, lambda e: e.tensor_tensor(out=o, in0=a, in1=b, op=op), reads=[a, b], writes=[o])

    def ts(eng, o, a, s1, s2, op0, op1=None, accum=None):
        kw = {}
        if op1 is not None:
            kw['op1'] = op1
        if accum is not None:
            kw['accum_out'] = accum
        wr = [o] + ([accum] if accum is not None else [])
        P.add(eng, lambda e: e.tensor_scalar(out=o, in0=a, scalar1=s1, scalar2=s2, op0=op0, **kw), reads=[a, s1, s2], writes=wr)

    def stt(eng, o, a, sc, b, op0, op1):
        P.add(eng, lambda e: e.scalar_tensor_tensor(out=o, in0=a, scalar=sc, in1=b, op0=op0, op1=op1), reads=[a, sc, b], writes=[o])

    def cp(eng, o, a):
        if eng == 'act':
            act(o, a, ACT.Copy)
        else:
            P.add(eng, lambda e: e.tensor_copy(out=o, in_=a), reads=[a], writes=[o])

    def memset(eng, o, val):
        P.add(eng, lambda e: e.memset(o, val), writes=[o])

    def recip(o, a):
        P.add('dve', lambda e: e.reciprocal(out=o, in_=a), reads=[a], writes=[o])

    dma_rr = {'n': 0}

    def dma(q, o, i, reads=(), writes=(), chan=None):
        if chan is None:
            chan = f"g{dma_rr['n'] % 6}"
            dma_rr['n'] += 1
        P.add(q, lambda e: e.dma_start(out=o, in_=i), reads=list(reads), writes=list(writes), chan=chan)
        return chan

    dma('pool', spk[:], sp_in, writes=[spk[:]])
    dma('pool', cst[:], cs_in, writes=[cst[:]])
    m0 = ar.mark()
    gst = ar.alloc([2 * 8, 128], F32)
    dma('pool', gst, gates_in.rearrange("p a n e -> p (a n) e"), writes=[gst])
    cp('act', identb[:], ident)
    cp('dve', permb[:], csf('perm'))
    cp('dve', trib[:], csf('triu'))
    memset('dve', onesb[:], 1.0)
    cp('act', gatesb[:].rearrange("p a n e -> p (a n) e"), gst)
    act(clam[:, 0:8], spf('lam'), ACT.Exp, scale=-1.0)
    act(clam[:, 0:8], clam[:, 0:8], ACT.Ln, bias=1.0)
    ts('dve', clam[:, 8:16], clam[:, 0:8], -16.0, None, ALU.mult)
    ts('dve', clam[:, 0:8], clam[:, 0:8], -8.0, None, ALU.mult)
    ar.reset(m0)

    cast_done = set()
    wcnt = {'n': 0, 'c': 0}

    def cast_unit(key, dst, src):
        if key in cast_done:
            return
        cast_done.add(key)
        ch = f"cast{wcnt['c'] % 8}"
        wcnt['c'] += 1
        P.add('pool', lambda e: e.dma_start(out=dst, in_=src), writes=[key], chan=ch)

    def load_unit(key, src_scr, shape3):
        slot = wcnt['n'] % NSLOT
        wcnt['n'] += 1
        a, b = shape3
        dst = ring[:, slot, 0:a * b]
        P.add('sp', lambda e: e.dma_start(out=dst, in_=src_scr), reads=[key], writes=[dst], chan=f"w{slot}")
        return dst.rearrange("p (a b) -> p a b", a=a)

    def unit_A(name, wsrc, scr, u):
        key = f"{name}:{u}"
        src = wsrc[:, u * 256:(u + 1) * 256].rearrange("(k p) j -> p k j", p=128)
        cast_unit(key, scr[u].rearrange("p (k j) -> p k j", k=KC), src)
        return load_unit(key, scr[u], (KC, 256))

    def unit_oin_last():
        key = "oin:16"
        if key not in cast_done:
            cast_done.add(key)
            dstc = s_oin[16].rearrange("p (k j) -> p k j", k=KC)
            ki_src = w_oin[:, 4096:4160].rearrange("(k p) j -> p k j", p=128)
            wi_src = w_oin[:, 4160:4176].rearrange("(k p) j -> p k j", p=128)
            P.add('pool', lambda e: e.dma_start(out=dstc[:, :, 0:64], in_=ki_src), writes=[key + 'a'], chan="castx0")
            P.add('pool', lambda e: e.dma_start(out=dstc[:, :, 64:128], in_=ki_src), writes=[key + 'b'], chan="castx1")
            P.add('pool', lambda e: e.dma_start(out=dstc[:, :, 128:144], in_=wi_src), writes=[key + 'c'], chan="castx2")
        slot = wcnt['n'] % NSLOT
        wcnt['n'] += 1
        dst = ring[:, slot, :].rearrange("p (a b) -> p a b", a=KC)
        srcv = s_oin[16].rearrange("p (k j) -> p k j", k=KC)
        P.add('sp', lambda e: e.dma_start(out=dst[:, :, 0:144], in_=srcv[:, :, 0:144]), reads=[key + 'a', key + 'b', key + 'c'],
              writes=[dst[:, :, 0:144]], chan=f"w{slot}")
        return dst

    def unit_down(l, m, hf):
        key = f"dn{l}:{m}:{hf}"
        src = w_dn[l, hf * 22 * 128:(hf + 1) * 22 * 128, m * 128:(m + 1) * 128].rearrange("(c p) j -> p c j", p=128)
        cast_unit(key, s_dn[l, m * 2 + hf].rearrange("p (c j) -> p c j", c=22), src)
        return load_unit(key, s_dn[l, m * 2 + hf], (22, 128))

    def rmsnorm(gain, hT, scr, ss_ready=False):
        ss = ssb
        if not ss_ready:
            for c in range(KC):
                sq = scr['sq'][c % 2]
                act(sq, xres[:, c, :], ACT.Square)
                mm(ss[:], onesb[:], sq, start=(c == 0), stop=(c == KC - 1))
        rs = scr['rstd']
        ts('dve', rs, ss[:], 1.0 / D, EPS, ALU.mult, ALU.add)
        act(rs, rs, ACT.Sqrt)
        recip(rs, rs)
        for c in range(KC):
            stt('dve', hT[:, c, :], xres[:, c, :], gain[:, c:c + 1], rs, ALU.mult, ALU.mult)

    def proj_fm(wu, j, hT, nk=KC):
        ps = psum.next()
        for k in range(nk):
            mm(ps[:], wu[:, k, j * 128:(j + 1) * 128], hT[:, k, :], start=(k == 0), stop=(k == nk - 1))
        return ps

    class SSFuse:
        def __init__(self):
            self.m0 = ar.mark()
            self.sq = Rot([ar.alloc([1, T], BF16)[:, 0, :] for _ in range(4)])
            self.pend = []

        def chunk_done(self, m):
            sq = self.sq.next()
            act(sq, xres[:, m, :], ACT.Square)
            self.pend.append((m, sq))
            if len(self.pend) > 2:
                self.flush1()

        def flush1(self):
            m, sq = self.pend.pop(0)
            mm(ssb[:], onesb[:], sq, start=(m == 0), stop=(m == KC - 1))

        def finish(self):
            while self.pend:
                self.flush1()
            ar.reset(self.m0)

    def residual_proj(name, wsrc, scr, nunits, src_fm, fuse=True):
        sf = SSFuse() if fuse else None
        for u in range(nunits):
            wu = unit_A(name, wsrc, scr, u)
            for j in range(2):
                m = 2 * u + j
                ps = proj_fm(wu, j, src_fm)
                tt('dve', xres[:, m, :], xres[:, m, :], ps[:], ALU.add)
                if sf:
                    sf.chunk_done(m)
        if sf:
            sf.finish()

    def ffn(l, st, fuse_next=False, store=None):
        m1 = ar.mark()
        hT = ar.alloc([KC, T], BF16)
        Abuf = ar.alloc([FC, T], BF16)
        scr = {'sq': [ar.alloc([1, T], BF16)[:, 0, :] for _ in range(2)], 'rstd': ar.alloc([1, T], F32)[:, 0, :]}
        gsb = [ar.alloc([1, T + 2], F32)[:, 0, :] for _ in range(2)]
        acc = [ar.alloc([1, T], F32)[:, 0, :] for _ in range(2)]
        sil = [ar.alloc([1, T], F32)[:, 0, :] for _ in range(2)]
        gain = spf('nffn', l * 16, l * 16 + 16)
        rmsnorm(gain, hT, scr, ss_ready=True)
        fwb = SP_OFF['fw'] + l * FC * 3
        fbb = SP_OFF['fb'] + l * FC
        ghalo = st['ghalo'][l]
        for i in range(N_GU // 2):
            wg = unit_A(f"gu{l}", w_gu[l], s_gu[l], i)
            wuu = unit_A(f"gu{l}", w_gu[l], s_gu[l], N_GU // 2 + i)
            for j in range(2):
                c = 2 * i + j
                psG = proj_fm(wg, j, hT)
                psU = proj_fm(wuu, j, hT)
                g = gsb[c % 2]
                a = acc[c % 2]
                s_ = sil[c % 2]
                cp('dve', g[:, 0:2], ghalo[:, c, :])
                act(g[:, 2:T + 2], psG[:], ACT.Copy)
                cp('act', ghalo[:, c, :], g[:, T:T + 2])
                ts('dve', a, g[:, 0:T], spk[:, fwb + c * 3:fwb + c * 3 + 1], spk[:, fbb + c:fbb + c + 1], ALU.mult, ALU.add)
                stt('dve', a, g[:, 1:T + 1], spk[:, fwb + c * 3 + 1:fwb + c * 3 + 2], a, ALU.mult, ALU.add)
                stt('dve', a, g[:, 2:T + 2], spk[:, fwb + c * 3 + 2:fwb + c * 3 + 3], a, ALU.mult, ALU.add)
                act(s_, a, ACT.Silu)
                tt('dve', Abuf[:, c, :], psU[:], s_, ALU.mult)
        sf = SSFuse() if fuse_next else None
        for m in range(KC):
            ps = psum.next()
            for hf in range(2):
                wd = unit_down(l, m, hf)
                for c2 in range(22):
                    mm(ps[:], wd[:, c2, :], Abuf[:, hf * 22 + c2, :], start=(hf == 0 and c2 == 0), stop=(hf == 1 and c2 == 21))
            tt('dve', xres[:, m, :], xres[:, m, :], ps[:], ALU.add)
            if sf:
                sf.chunk_done(m)
            if store is not None:
                dma('pool', store[0][:, m * T:(m + 1) * T], xres[:, m, :], reads=[xres[:, m, :]], writes=[store[1]], chan=f"x2st{m % 2}")
        if sf:
            sf.finish()
        ar.reset(m1)

    mA = ar.mark()
    stA = {}
    stA['R'] = ar.alloc([8, 128], F32)
    stA['Sbf'] = ar.alloc([8, 128], BF16)
    stA['xhalo'] = ar.alloc([8, 3], F32)
    stA['hst'] = ar.alloc([1, 8], F32)[:, 0, :]
    stA['ghalo'] = [ar.alloc([FC, 2], F32), None]
    stA['rot'] = ar.alloc([2, T], F32)
    mA2 = ar.mark()

    def layer0_tile(sq_i, ti):
        st = stA
        m1 = ar.mark()
        stg = [ar.alloc([1, D], F32)[:, 0, :] for _ in range(2)]
        for sub in range(NSUB):
            sg = stg[sub % 2]
            r0 = ti * T + sub * 128
            dma('pool', sg, x_in[sq_i, r0:r0 + 128, :], writes=[sg], chan=f"xl{sub % 2}")
            for c4 in range(4):
                ps = psum.next()
                for cc in range(4):
                    c = c4 * 4 + cc
                    tr(ps[:, cc * 128:(cc + 1) * 128], sg[:, c * 128:(c + 1) * 128], ident)
                cp('act' if c4 % 2 == 0 else 'dve', xres[:, c4 * 4:c4 * 4 + 4, sub * 128:(sub + 1) * 128],
                   ps[:].rearrange("p (a b) -> p a b", a=4))
        ar.reset(m1)
        dma('pool', st['rot'], rot_in[:, :, ti * T:(ti + 1) * T].rearrange("a p t -> p a t"), writes=[st['rot']], chan="rot")
        cosT = st['rot'][:, 0, :]
        sinS = st['rot'][:, 1, :]
        hT = ar.alloc([KC, T], BF16)
        mixed = ar.alloc([KC, T], BF16)
        scr = {'sq': [ar.alloc([1, T], BF16)[:, 0, :] for _ in range(2)], 'rstd': ar.alloc([1, T], F32)[:, 0, :]}
        if stop == 'load':
            ar.reset(m1)
            return
        rmsnorm(spf('nmix', 0, 16), hT, scr)
        if stop == 'norm':
            ar.reset(m1)
            return
        m2 = ar.mark()
        xr = ar.alloc([8, T + 3], F32)
        gy = ar.alloc([8, T], BF16)
        f32s = Rot([ar.alloc([1, T], F32)[:, 0, :] for _ in range(10)])
        xcbs = Rot([ar.alloc([1, T], BF16)[:, 0, :] for _ in range(3)])
        if ti == 0:
            memset('dve', st['xhalo'], 0.0)
            memset('dve', st['hst'], 0.0)
        for n in range(8):
            cp('dve', xr[:, n, 0:3], st['xhalo'][:, n, :])
        def lru_stage2(n, xc, xcb):
            psa = psum.next()
            mm(psa[:], gatesb[:, 0, n, :], xcb)
            psi = psum.next()
            mm(psi[:], gatesb[:, 1, n, :], xcb)
            r_ = f32s.next()
            act(r_, psa[:], ACT.Sigmoid, bias=spf('ba', n, n + 1))
            i_ = f32s.next()
            act(i_, psi[:], ACT.Sigmoid, bias=spf('bx', n, n + 1))
            a_ = f32s.next()
            act(a_, r_, ACT.Exp, scale=clam[:, n:n + 1])
            a2 = f32s.next()
            act(a2, r_, ACT.Exp, scale=clam[:, 8 + n:9 + n])
            ts('dve', a2, a2, -1.0, 1.0, ALU.mult, ALU.add)
            ts('dve', a2, a2, 0.0, None, ALU.max)
            act(a2, a2, ACT.Sqrt)
            tt('dve', i_, i_, xc, ALU.mult)
            tt('dve', i_, i_, a2, ALU.mult)
            P.add('dve', lambda e: e.tensor_tensor_scan(out=a_, data0=a_, data1=i_, initial=st['hst'][:, n:n + 1],
                                                        op0=ALU.mult, op1=ALU.add),
                  reads=[a_, i_, st['hst'][:, n:n + 1]], writes=[a_])
            cp('act', st['hst'][:, n:n + 1], a_[:, T - 1:T])
            tt('dve', mixed[:, 8 + n, :], a_, gy[:, n, :], ALU.mult)

        prev_blk = None
        for u4 in range(4):
            wx_ = unit_A("ein", w_ein, s_ein, 16 + u4)
            wy_ = unit_A("ein", w_ein, s_ein, 20 + u4)
            for j in range(2):
                n = 2 * u4 + j
                psx = proj_fm(wx_, j, hT)
                act(xr[:, n, 3:T + 3], psx[:], ACT.Copy)
                psy = proj_fm(wy_, j, hT)
                act(gy[:, n, :], psy[:], ACT.Gelu_apprx_tanh)
                cp('act', st['xhalo'][:, n, :], xr[:, n, T:T + 3])
                cwb = SP_OFF['cw'] + n * 4
                xc = f32s.next()
                ts('dve', xc, xr[:, n, 0:T], spk[:, cwb:cwb + 1], spf('cb', n, n + 1), ALU.mult, ALU.add)
                for jj in range(1, 4):
                    stt('dve', xc, xr[:, n, jj:jj + T], spk[:, cwb + jj:cwb + jj + 1], xc, ALU.mult, ALU.add)
                xcb = xcbs.next()
                cp('act', xcb, xc)
                if prev_blk is not None:
                    lru_stage2(*prev_blk)
                prev_blk = (n, xc, xcb)
        lru_stage2(*prev_blk)
        ar.reset(m2)
        if stop == 'lru':
            ar.reset(m1)
            return
        qd = ar.alloc([8, T], BF16)
        kd = ar.alloc([8, T], BF16)
        kdt = ar.alloc([NSUB, 1024], BF16)
        vt = ar.alloc([NSUB, 1024], BF16)
        Gs = ar.alloc([8, T], BF16)
        qsbs = Rot([ar.alloc([1, T], BF16)[:, 0, :] for _ in range(2)])
        ysqs = Rot([ar.alloc([1, T], BF16)[:, 0, :] for _ in range(2)])
        f32r = Rot([ar.alloc([1, T], F32)[:, 0, :] for _ in range(6)])
        STs = Rot([ar.alloc([1, T], BF16)[:, 0, :] for _ in range(2)])
        if ti == 0 and not os.environ.get('K_NOMEMSET'):
            memset('dve', st['R'], 0.0)
            memset('dve', st['Sbf'], 0.0)

        def rotary(ps, dst, dec):
            qs = qsbs.next()
            t1 = f32r.next()
            t2 = f32r.next()
            tt('dve', t1, ps[:], cosT, ALU.mult)
            P.add('act', lambda e: e.activation(out=qs, in_=ps[:], func=ACT.Copy), reads=[ps[:], t1], writes=[qs])
            if not os.environ.get('K_NOPERM'):
                ps2 = psum.next()
                mm(ps2[:], permb[:], qs)
                tt('dve', t2, ps2[:], sinS, ALU.mult)
                tt('dve', t1, t1, t2, ALU.add)
            if os.environ.get('K_NOBC'):
                cp('dve', dst, t1)
                return
            tt('dve', dst.rearrange("p (a b) -> p a b", a=4), t1.rearrange("p (a b) -> p a b", a=4),
               dec.unsqueeze(1).to_broadcast([128, 4, 128]), ALU.mult)

        for hb in range(2):
            for uu in range(2):
                wq = unit_A("ein", w_ein, s_ein, hb * 2 + uu)
                for j in range(2):
                    h = hb * 4 + uu * 2 + j
                    ps = proj_fm(wq, j, hT)
                    rotary(ps, qd[:, h, :], csf('qdec', h * 128, (h + 1) * 128))
            for uu in range(2):
                wk = unit_A("ein", w_ein, s_ein, 4 + hb * 2 + uu)
                for j in range(2):
                    h = hb * 4 + uu * 2 + j
                    ps = proj_fm(wk, j, hT)
                    rotary(ps, kd[:, h, :], csf('kdec', h * 128, (h + 1) * 128))
                    pst = psum.next()
                    for sub in range(NSUB):
                        mm(pst[:, sub * 128:(sub + 1) * 128], kd[:, h, sub * 128:(sub + 1) * 128], identb[:])
                    cp('act', kdt[:, :, h * 128:(h + 1) * 128], pst[:].rearrange("p (a b) -> p a b", a=4))
            for uu in range(2):
                wv = unit_A("ein", w_ein, s_ein, 8 + hb * 2 + uu)
                for sub in range(NSUB):
                    ps = psum.next()
                    for k in range(KC):
                        mm(ps[:, 0:256], hT[:, k, sub * 128:(sub + 1) * 128], wv[:, k, :], start=(k == 0), stop=(k == KC - 1))
                    c_lo = (hb * 2 + uu) * 256
                    cp('act' if sub % 2 == 0 else 'dve', vt[:, sub, c_lo:c_lo + 256], ps[:, 0:256])
            for uu in range(2):
                wg_ = unit_A("ein", w_ein, s_ein, 12 + hb * 2 + uu)
                for j in range(2):
                    h = hb * 4 + uu * 2 + j
                    ps = proj_fm(wg_, j, hT)
                    sl = f32r.next()
                    act(sl, ps[:], ACT.Silu)
                    ts('dve', Gs[:, h, :], sl, spf('gn', h, h + 1), None, ALU.mult)
        for n in range(NSUB):
            cols = slice(n * 128, (n + 1) * 128)
            STb = [None, None]
            psY = [None, None]
            ysq = [None, None]
            for hb in range(2):
                psS = psum.next()
                for hl in range(4):
                    h = hb * 4 + hl
                    mm(psS[:, hl * 128:(hl + 1) * 128], kd[:, h, cols], qd[:, h, cols])
                psKV = psum.next()
                for hl in range(4):
                    h = hb * 4 + hl
                    mm(psKV[:, hl * 128:(hl + 1) * 128], kdt[:, n, h * 128:(h + 1) * 128], vt[:, n, h * 128:(h + 1) * 128])
                STb[hb] = STs.next()
                tt('dve', STb[hb].rearrange("p (a b) -> p a b", a=4), psS[:].rearrange("p (a b) -> p a b", a=4),
                   csf('triu').unsqueeze(1).to_broadcast([128, 4, 128]), ALU.mult)
                STb[hb] = (STb[hb], psKV)
            for hb in range(2):
                stb, psKV = STb[hb]
                psY[hb] = psum.next()
                for hl in range(4):
                    h = hb * 4 + hl
                    mm(psY[hb][:, hl * 128:(hl + 1) * 128], vt[:, n, h * 128:(h + 1) * 128], stb[:, hl * 128:(hl + 1) * 128], start=True, stop=False)
                    mm(psY[hb][:, hl * 128:(hl + 1) * 128], st['Sbf'][:, h, :], qd[:, h, cols], start=False, stop=True)
                for hl in range(4):
                    h = hb * 4 + hl
                    stt('dve', st['R'][:, h, :], st['R'][:, h, :], float(CDEC[h]), psKV[:, hl * 128:(hl + 1) * 128], ALU.mult, ALU.add)
                    act(st['Sbf'][:, h, :], st['R'][:, h, :], ACT.Copy, scale=float(CDEC[h]))
                ysq[hb] = ysqs.next()
                act(ysq[hb], psY[hb][:], ACT.Square)
            for hb in range(2):
                psN = psum.next()
                mm(psN[:], onesb[:], ysq[hb])
                rs = f32r.next()
                ts('dve', rs, psN[:], 1.0 / 128, EPS, ALU.mult, ALU.add)
                act(rs, rs, ACT.Sqrt)
                recip(rs, rs)
                yn = f32r.next()
                tt('dve', yn, psY[hb][:], rs, ALU.mult)
                tt('dve', mixed[:, hb * 4:hb * 4 + 4, cols], yn.rearrange("p (a b) -> p a b", a=4), Gs[:, hb * 4:hb * 4 + 4, cols], ALU.mult)
        if stop == 'ret':
            ar.reset(m1)
            return
        residual_proj("eout", w_eout, s_eout, N_EOUT, mixed)
        ar.reset(m1)

    def alloc_passB():
        st = {}
        st['Kc'] = ar.alloc([4, S], BF16)
        st['Vc'] = ar.alloc([S // 128, 512], BF16)
        st['ki2'] = ar.alloc([1, S], BF16)[:, 0, :]
        st['ghalo'] = [None, ar.alloc([FC, 2], F32)]
        st['bt'] = [ar.alloc([1, 256], F32)[:, 0, :] for _ in range(2)]
        return st

    def layer1_tile(st, sq_i, ti):
        m1 = ar.mark()
        selT = ar.alloc([S // 128, T], BF16)
        qF = ar.alloc([16, T], BF16)
        m_h = ar.mark()
        hT_lo = ar.mark()
        hT = ar.alloc([KC, T], BF16)
        attn = hT
        qiF = ar.alloc([8, T], BF16)
        wi = ar.alloc([NSUB, 16], F32)
        scr = {'sq': [ar.alloc([1, T], BF16)[:, 0, :] for _ in range(2)], 'rstd': ar.alloc([1, T], F32)[:, 0, :]}
        rmsnorm(spf('nmix', 16, 32), hT, scr)
        tcols = slice(ti * T, (ti + 1) * T)
        for u in range(8):
            wq = unit_A("oin", w_oin, s_oin, u)
            for j in range(2):
                h = 2 * u + j
                ps = proj_fm(wq, j, hT)
                act(qF[:, h, :], ps[:], ACT.Copy, scale=128.0 ** -0.5)
        for u in range(2):
            wk = unit_A("oin", w_oin, s_oin, 8 + u)
            for j in range(2):
                kvh = 2 * u + j
                ps = proj_fm(wk, j, hT)
                cp('dve', st['Kc'][:, kvh, tcols], ps[:])
        for u in range(2):
            wv = unit_A("oin", w_oin, s_oin, 10 + u)
            for sub in range(NSUB):
                ps = psum.next()
                for k in range(KC):
                    mm(ps[:, 0:256], hT[:, k, sub * 128:(sub + 1) * 128], wv[:, k, :], start=(k == 0), stop=(k == KC - 1))
                cp('act' if sub % 2 == 0 else 'dve', st['Vc'][:, ti * NSUB + sub, u * 256:(u + 1) * 256], ps[:, 0:256])
        for u in range(4):
            wqi = unit_A("oin", w_oin, s_oin, 12 + u)
            for j in range(2):
                cq = 2 * u + j
                ps = proj_fm(wqi, j, hT)
                cp('act', qiF[:, cq, :], ps[:])
        wl = unit_oin_last()
        ps = proj_fm(wl, 0, hT)
        cp('dve', st['ki2'][:, tcols], ps[:])
        for sub in range(NSUB):
            ps = psum.next()
            for k in range(KC):
                mm(ps[:, 0:16], hT[:, k, sub * 128:(sub + 1) * 128], wl[:, k, 128:144], start=(k == 0), stop=(k == KC - 1))
            act(wi[:, sub, :], ps[:, 0:16], ACT.Copy, scale=1.0 / 32.0)
        m2 = ar.mark()
        Lt = T * (ti + 1)
        na = Lt // 128
        scoreA = ar.alloc([1, S], F32)[:, 0, :]
        scoreB = arena[:, hT_lo // 2:(hT_lo + 4 * S) // 2].bitcast(F32)
        selb = [ar.alloc([1, S], BF16)[:, 0, :] for _ in range(2)]
        junk = selb[1]
        Rb = Rot([ar.alloc([1, T], F32)[:, 0, :] for _ in range(3)])
        smb = [ar.alloc([1, 8 + NIT], F32)[:, 0, :] for _ in range(2)]
        todo = []
        for sub in range(NSUB):
            gt = ti * NSUB + sub
            scol = slice(sub * 128, (sub + 1) * 128)
            if gt < 2:
                for a in range(na):
                    dst = selT[:, a, scol]
                    if a < gt:
                        cp('act', dst, onesb[:])
                    elif a == gt:
                        cp('act', dst, trib[:])
                    else:
                        memset('dve', dst, 0.0)
            else:
                todo.append(sub)
        for p0 in range(0, len(todo), 2):
            pair = todo[p0:p0 + 2]
            scs = [scoreA, scoreB]
            Ls = []
            for ix, sub in enumerate(pair):
                gt = ti * NSUB + sub
                scol = slice(sub * 128, (sub + 1) * 128)
                score = scs[ix]
                sm = smb[ix]
                L = 128 * (gt + 1)
                Ls.append(L)
                nblk = (L + 511) // 512
                for sbk in range(nblk):
                    w = min(512, L - 512 * sbk)
                    blk = slice(sbk * 512, sbk * 512 + w)
                    for h in range(16):
                        cq = h // 2
                        rows = slice((h % 2) * 64, (h % 2) * 64 + 64)
                        ps = psum.next()
                        mm(ps[:, 0:w], qiF[rows, cq, scol], st['ki2'][rows, blk])
                        rb = Rb.next()
                        act(rb[:, 0:w], ps[:, 0:w], ACT.Relu)
                        if h == 0:
                            ts('dve', score[:, blk], rb[:, 0:w], wi[:, sub, 0:1], None, ALU.mult)
                        else:
                            stt('dve', score[:, blk], rb[:, 0:w], wi[:, sub, h:h + 1], score[:, blk], ALU.mult, ALU.add)
                ts('dve', junk[:, 0:L], score[:, 0:L], 1.0, None, ALU.mult, ALU.max, accum=sm[:, 0:1])
                ts('dve', junk[:, 0:L], score[:, 0:L], 1.0, None, ALU.mult, ALU.min, accum=sm[:, 1:2])
                tt('dve', score[:, L - 128:L], score[:, L - 128:L], csf('negm'), ALU.add)
                if L < Lt:
                    memset('dve', score[:, L:Lt], NEG)
                tt('dve', sm[:, 5:6], sm[:, 0:1], sm[:, 1:2], ALU.subtract)
                ts('dve', sm[:, 8:8 + NIT], csf('pow2'), sm[:, 5:6], None, ALU.mult)
            for k in range(NIT):
                for ix in range(len(pair)):
                    sm = smb[ix]
                    tt('dve', sm[:, 2:3], sm[:, 1:2], sm[:, 8 + k:9 + k], ALU.add)
                for ix in range(len(pair)):
                    sm = smb[ix]
                    ts('dve', junk[:, 0:Ls[ix]], scs[ix][:, 0:Ls[ix]], sm[:, 2:3], None, ALU.is_ge, ALU.add, accum=sm[:, 3:4])
                for ix in range(len(pair)):
                    sm = smb[ix]
                    stt('dve', sm[:, 4:5], sm[:, 3:4], 256.0, sm[:, 8 + k:9 + k], ALU.is_ge, ALU.mult)
                for ix in range(len(pair)):
                    sm = smb[ix]
                    tt('dve', sm[:, 1:2], sm[:, 1:2], sm[:, 4:5], ALU.add)
            for ix, sub in enumerate(pair):
                scol = slice(sub * 128, (sub + 1) * 128)
                sel = selb[ix]
                ts('dve', sel[:, 0:Lt], scs[ix][:, 0:Lt], smb[ix][:, 1:2], None, ALU.is_ge)
                for a0 in range(0, na, 4):
                    n4 = min(4, na - a0)
                    pst = psum.next()
                    for a in range(n4):
                        mm(pst[:, a * 128:(a + 1) * 128], sel[:, (a0 + a) * 128:(a0 + a + 1) * 128], identb[:])
                    cp('act', selT[:, a0:a0 + n4, scol], pst[:, 0:n4 * 128].rearrange("p (a b) -> p a b", a=n4))
        ar.reset(m2)
        Eb = Rot([ar.alloc([1, T], BF16)[:, 0, :] for _ in range(3)])
        Pm = Rot([ar.alloc([1, T], BF16)[:, 0, :] for _ in range(4)])
        tf = Rot([ar.alloc([1, 256], F32)[:, 0, :] for _ in range(2)])
        rz = Rot([ar.alloc([1, T], F32)[:, 0, :] for _ in range(2)])
        acc_banks = Rot(psb[0:4])
        tmp_banks = Rot(psb[4:8])

        def flush_one(pend, psO, psZ, kvh):
            a_, c0_, pm_ = pend.pop(0)
            mm(psO[:, c0_:T], st['Vc'][:, a_, kvh * 128:(kvh + 1) * 128], pm_[:, c0_:T], start=(a_ == 0), stop=(a_ == na - 1))
            mm(psZ[:, c0_:T], onesb[:], pm_[:, c0_:T], start=(a_ == 0), stop=(a_ == na - 1))
        for hq in range(16):
            kvh = hq // 4
            btb = st['bt'][hq % 2]
            dma('pool', btb, bt_in[hq], writes=[btb], chan=f"bt{hq % 2}")
            psO = acc_banks.next()
            psZ = acc_banks.next()
            pend = []
            for a in range(na):
                sa = a - ti * NSUB
                c0 = max(0, sa) * 128
                psL = tmp_banks.next()
                mm(psL[:, c0:T], st['Kc'][:, kvh, a * 128:(a + 1) * 128], qF[:, hq, c0:T])
                eb = Eb.next()
                n1 = c0
                if sa >= -1:
                    n0 = max(0, sa) * 128
                    n1 = min(NSUB, sa + 2) * 128
                    tfb = tf.next()
                    tt('dve', tfb[:, 0:n1 - n0], psL[:, n0:n1], btb[:, n0 - 128 * sa:n1 - 128 * sa], ALU.add)
                    act(eb[:, n0:n1], tfb[:, 0:n1 - n0], ACT.Exp)
                if n1 < T:
                    act(eb[:, n1:T], psL[:, n1:T], ACT.Exp, bias=csf('c31', hq, hq + 1))
                pm = Pm.next()
                tt('dve', pm[:, c0:T], eb[:, c0:T], selT[:, a, c0:T], ALU.mult)
                pend.append((a, c0, pm))
                if len(pend) > 2:
                    flush_one(pend, psO, psZ, kvh)
            while pend:
                flush_one(pend, psO, psZ, kvh)
            r = rz.next()
            recip(r, psZ[:])
            tt('dve', attn[:, hq, :], psO[:], r, ALU.mult)
        residual_proj("oout", w_oout, s_oout, N_OOUT, attn)
        ar.reset(m1)

    def final_tile(sq_i, ti):
        m1 = ar.mark()
        scr = {'sq': [ar.alloc([1, T], BF16)[:, 0, :] for _ in range(2)], 'rstd': ar.alloc([1, T], F32)[:, 0, :]}
        ostg = [ar.alloc([1, D], F32)[:, 0, :] for _ in range(2)]
        ss = ssb
        rs = scr['rstd']
        ts('dve', rs, ss[:], 1.0 / D, EPS, ALU.mult, ALU.add)
        act(rs, rs, ACT.Sqrt)
        recip(rs, rs)
        for c in range(KC):
            stt('dve', xres[:, c, :], xres[:, c, :], spf('fnorm', c, c + 1), rs, ALU.mult, ALU.mult)
        chs = []
        for sub in range(NSUB):
            og = ostg[sub % 2]
            for c4 in range(4):
                ps = psum.next()
                for cc in range(4):
                    c = c4 * 4 + cc
                    tr(ps[:, cc * 128:(cc + 1) * 128], xres[:, c, sub * 128:(sub + 1) * 128], ident)
                cp('act' if c4 % 2 == 0 else 'dve', og[:, c4 * 512:(c4 + 1) * 512], ps[:])
            r0 = ti * T + sub * 128
            chs.append(dma('pool', out[sq_i, r0:r0 + 128, :], og, reads=[og], chan=f"os{sub % 2}"))
        ar.reset(m1)
        return chs

    final_chans = set()
    for sq_i in range(nseq):
        ar.reset(mA2)
        for ti in range(NTL):
            if ti == 0:
                memset('dve', stA['ghalo'][0], 0.0)
            layer0_tile(sq_i, ti)
            key = f"x2:{sq_i}:{ti}"
            ffn(0, stA, store=(x2s[sq_i, ti], key))
            if debug_stage == 'A':
                final_chans.add("x2st0")
                final_chans.add("x2st1")
        if debug_stage == 'A':
            continue
        ar.reset(mA)
        stB = alloc_passB()
        for ti in range(NT):
            key = f"x2:{sq_i}:{ti}"
            dma('pool', xres[:].rearrange("p a b -> p (a b)"), x2s[sq_i, ti], reads=[key], writes=[xres[:]], chan="x2ld")
            if ti == 0:
                memset('dve', stB['ghalo'][1], 0.0)
            layer1_tile(stB, sq_i, ti)
            ffn(1, stB, fuse_next=True)
            for ch in final_tile(sq_i, ti):
                final_chans.add(ch)
        ar.reset(mA)
        ar.reset(mA2)
    P.emit(final_chans=sorted(final_chans))
    return nc, P


def _fm(v, nchunk):
    return np.ascontiguousarray(np.asarray(v, np.float32).reshape(nchunk, 128).T)


def _t5_bucket(n):
    n = np.maximum(n, 0)
    nf = np.maximum(n, 16).astype(np.float32)
    large = 16 + (np.log(nf / np.float32(16)) / np.float32(math.log(128 / 16)) * np.float32(16)).astype(np.int32)
    large = np.minimum(large, 31)
    return np.where(n < 16, n, large)


def host_prep(inp):
    f = np.float32
    sp = np.zeros((128, SP_W), f)

    def put(name, arr):
        arr = np.asarray(arr, f).reshape(128, -1)
        sp[:, SP_OFF[name]:SP_OFF[name] + arr.shape[1]] = arr

    put('nmix', np.stack([_fm(inp['norm_mix'][l], 16) for l in range(2)], 1))
    put('nffn', np.stack([_fm(inp['norm_ffn'][l], 16) for l in range(2)], 1))
    put('fnorm', _fm(inp['final_norm'], 16))
    put('gn', _fm(inp['e_ret_gn'][0], 8))
    put('cw', np.asarray(inp['e_conv_w'][0], f).reshape(4, 8, 128).transpose(2, 1, 0))
    put('cb', _fm(inp['e_conv_b'][0], 8))
    put('ba', _fm(inp['e_gate_a_b'][0], 8))
    put('bx', _fm(inp['e_gate_x_b'][0], 8))
    put('lam', _fm(inp['e_lambda'][0], 8))
    put('fw', np.asarray(inp['ffn_conv_w'], f).reshape(2, 3, FC, 128).transpose(3, 0, 2, 1))
    put('fb', np.asarray(inp['ffn_conv_b'], f).reshape(2, FC, 128).transpose(2, 0, 1))

    cs = np.zeros((128, CS_W), f)

    def putc(name, arr):
        arr = np.asarray(arr, f).reshape(128, -1)
        cs[:, CS_OFF[name]:CS_OFF[name] + arr.shape[1]] = arr

    idx = np.arange(128)
    putc('ident', np.eye(128, dtype=f))
    putc('triu', (idx[None, :] >= idx[:, None]).astype(f))
    perm = np.zeros((128, 128), f)
    perm[(idx + 64) % 128, idx] = 1.0
    putc('perm', perm)
    putc('negm', np.where(idx[None, :] > idx[:, None], f(NEG), f(0)))
    g64 = np.array(GAMMA, np.float64)
    i64 = np.arange(128, dtype=np.float64)
    qdec = g64[:, None] ** (i64[None, :] + 1.0)
    kdec = g64[:, None] ** (-(i64[None, :] + 1.0)) * (128.0 ** -0.5)
    putc('qdec', np.broadcast_to(qdec.reshape(1, 1024), (128, 1024)))
    putc('kdec', np.broadcast_to(kdec.reshape(1, 1024), (128, 1024)))
    putc('c31', np.broadcast_to(np.asarray(inp['rel_bias'], f)[31][None, :], (128, 16)))
    putc('pow2', np.broadcast_to((2.0 ** (-(np.arange(NIT, dtype=np.float64) + 1.0)))[None, :], (128, NIT)))

    gates = np.stack([np.asarray(inp['e_gate_a_w'][0], f).transpose(1, 0, 2),
                      np.asarray(inp['e_gate_x_w'][0], f).transpose(1, 0, 2)], 1)
    half = 64
    freqs = (f(10000.0) ** (-np.arange(half, dtype=f) / f(half))).astype(f)
    ang = (np.arange(S, dtype=f)[:, None] * freqs[None, :]).astype(f)
    cos = np.cos(ang).astype(f).T
    sin = np.sin(ang).astype(f).T
    rot = np.stack([np.concatenate([cos, cos], 0), np.concatenate([-sin, sin], 0)], 0)
    rel = idx[None, :256 - 128 + 128] if False else (np.arange(256)[None, :] - idx[:, None])
    bidx = _t5_bucket(rel)
    bt = np.ascontiguousarray(np.asarray(inp['rel_bias'], f)[bidx].transpose(2, 0, 1))
    return dict(smallp=sp, consts=cs, gates=np.ascontiguousarray(gates), rot=np.ascontiguousarray(rot), bt=bt)


_CACHE = {}


def kernel(**inputs):
    n_cores = 8
    inp = {k: np.asarray(v) for k, v in inputs.items()}
    hp = host_prep(inp)
    shared = dict(hp)
    shared['e_w_in'] = np.ascontiguousarray(inp['e_w_in'][0], dtype=np.float32)
    shared['e_w_out'] = np.ascontiguousarray(inp['e_w_out'][0], dtype=np.float32)
    shared['o_w_in'] = np.ascontiguousarray(inp['o_w_in'][0], dtype=np.float32)
    shared['o_w_out'] = np.ascontiguousarray(inp['o_w_out'][0], dtype=np.float32)
    shared['ffn_w_gu'] = np.ascontiguousarray(inp['ffn_w_gu'], dtype=np.float32)
    shared['ffn_w_down'] = np.ascontiguousarray(inp['ffn_w_down'], dtype=np.float32)
    x = np.ascontiguousarray(inp['x'], dtype=np.float32)
    if 'nc' not in _CACHE:
        _CACHE['nc'] = build_program()[0]
    nc = _CACHE['nc']
    in_maps = []
    for c in range(n_cores):
        m = dict(shared)
        m['x'] = x[c * NSEQ:(c + 1) * NSEQ]
        in_maps.append(m)
    res = run_bass_kernel_spmd(nc, in_maps, core_ids=list(range(n_cores)))
    return np.concatenate([r['out'] for r in res.results], axis=0)
```
